# Optimizing a Trainium2 kernel written in Bass

```python
import jax, jax.numpy as jnp
from jax import lax
import numpy as np

D_MODEL = 1024
BATCH = 2
SEQ = 8192
DEPTH = 1

HG_HEADS = 8
HG_DK = 128
HG_DV = 128
HG_CHUNK = 64
RET_HEADS = 4
RET_DK = 256
RET_DV = 512
RET_CHUNK = 128
ROPE_BASE = 10000.0
PEER_HEADS = 8
PEER_NKEYS = 128
PEER_N_EXPERTS = PEER_NKEYS * PEER_NKEYS
PEER_QDIM = 256
PEER_HALF = PEER_QDIM // 2
PEER_TOPK = 16
PEER_BLOCK = 128
NORM_EPS = 1e-6

IN_SIZES = (HG_HEADS * HG_DK, HG_HEADS * HG_DK, HG_HEADS * HG_DV, HG_HEADS * HG_DV,
            RET_HEADS * RET_DK, RET_HEADS * RET_DK, RET_HEADS * RET_DV, RET_HEADS * RET_DV,
            D_MODEL, D_MODEL)
IN_WIDTH = (2 * HG_HEADS * HG_DK + 2 * HG_HEADS * HG_DV + 2 * RET_HEADS * RET_DK
            + 2 * RET_HEADS * RET_DV + 2 * D_MODEL)

kernel_name = "hybrid_hgrn2_retnet_peer"


def rmsnorm(x, w):
    xf = x.astype(jnp.float32)
    y = xf * lax.rsqrt(jnp.mean(xf * xf, axis=-1, keepdims=True) + NORM_EPS)
    return (y * w.astype(jnp.float32)).astype(x.dtype)


def head_rms(x):
    return x * lax.rsqrt(jnp.mean(x * x, axis=-1, keepdims=True) + NORM_EPS)


def split_cols(t, sizes):
    outs, start = [], 0
    for n in sizes:
        outs.append(t[..., start:start + n])
        start += n
    return outs


def to_chunks(t, chunk):
    B, S, H, d = t.shape
    return t.reshape(B, S // chunk, chunk, H, d).transpose(1, 0, 3, 2, 4)


def from_chunks(t):
    nc, B, H, C, d = t.shape
    return t.transpose(1, 0, 3, 2, 4).reshape(B, nc * C, H, d)


def hgrn2_mix(q, f_logit, i, lb):
    B, S, _ = q.shape
    f = lb + (1.0 - lb) * jax.nn.sigmoid(f_logit)
    k = 1.0 - f
    logf = jnp.log(f)
    qc = to_chunks((q * HG_DK ** -0.5).reshape(B, S, HG_HEADS, HG_DK), HG_CHUNK)
    kc = to_chunks(k.reshape(B, S, HG_HEADS, HG_DK), HG_CHUNK)
    gc = to_chunks(logf.reshape(B, S, HG_HEADS, HG_DK), HG_CHUNK)
    vc = to_chunks(i.reshape(B, S, HG_HEADS, HG_DV), HG_CHUNK)
    causal = jnp.tril(jnp.ones((HG_CHUNK, HG_CHUNK), dtype=bool))

    def step(state, inp):
        qb, kb, gb, vb = inp
        b = jnp.cumsum(gb, axis=2)
        diff = b[:, :, :, None, :] - b[:, :, None, :, :]
        decay = jnp.exp(jnp.where(causal[:, :, None], diff, -jnp.inf))
        a = jnp.einsum('bhtd,bhsd,bhtsd->bhts', qb, kb, decay)
        o = (jnp.einsum('bhts,bhsv->bhtv', a, vb)
             + jnp.einsum('bhtd,bhdv->bhtv', qb * jnp.exp(b), state))
        b_last = b[:, :, -1:, :]
        state = (jnp.exp(b_last[:, :, 0, :])[..., None] * state
                 + jnp.einsum('bhsd,bhsv->bhdv', kb * jnp.exp(b_last - b), vb))
        return state, o

    s0 = jnp.zeros((B, HG_HEADS, HG_DK, HG_DV), jnp.float32)
    _, o = lax.scan(step, s0, (qc, kc, gc, vc))
    return from_chunks(o)


def rotary(x, pos):
    half = x.shape[-1] // 2
    inv = ROPE_BASE ** (-jnp.arange(half, dtype=jnp.float32) / half)
    ang = pos[:, None] * inv[None, :]
    cos = jnp.cos(ang)[None, :, None, :]
    sin = jnp.sin(ang)[None, :, None, :]
    x1, x2 = x[..., :half], x[..., half:]
    return jnp.concatenate([x1 * cos - x2 * sin, x1 * sin + x2 * cos], axis=-1)


def retention_mix(q, k, v):
    C = RET_CHUNK
    log_g = jnp.log(1.0 - jnp.exp2(-5.0 - jnp.arange(RET_HEADS, dtype=jnp.float32)))
    idx = jnp.arange(C, dtype=jnp.float32)
    rel = idx[:, None] - idx[None, :]
    dmask = jnp.where(rel >= 0, jnp.exp(log_g[:, None, None] * jnp.maximum(rel, 0.0)), 0.0)
    q_decay = jnp.exp(log_g[:, None] * (idx + 1.0))[None, :, :, None]
    k_decay = jnp.exp(log_g[:, None] * (C - 1.0 - idx))[None, :, :, None]
    chunk_decay = jnp.exp(log_g * C)[None, :, None, None]
    qc, kc, vc = to_chunks(q, C), to_chunks(k, C), to_chunks(v, C)

    def step(state, inp):
        qb, kb, vb = inp
        a = jnp.einsum('bhtd,bhsd->bhts', qb, kb) * dmask[None]
        o = (jnp.einsum('bhts,bhsv->bhtv', a, vb)
             + jnp.einsum('bhtd,bhdv->bhtv', qb * q_decay, state))
        state = chunk_decay * state + jnp.einsum('bhsd,bhsv->bhdv', kb * k_decay, vb)
        return state, o

    B = q.shape[0]
    s0 = jnp.zeros((B, RET_HEADS, RET_DK, RET_DV), jnp.float32)
    _, o = lax.scan(step, s0, (qc, kc, vc))
    return from_chunks(o)


def peer_ffn(h, w_q, sub_keys, expert_u, expert_v):
    B, S, D = h.shape
    T = B * S
    ht = h.reshape(T, D)
    q = (ht @ w_q).astype(jnp.float32).reshape(T, PEER_HEADS, 2, PEER_HALF)
    s = jnp.einsum('thpd,hpkd->thpk', q, sub_keys.astype(jnp.float32))
    top_s, top_i = lax.top_k(s, PEER_TOPK)
    cand = top_s[:, :, 0, :, None] + top_s[:, :, 1, None, :]
    cand_idx = top_i[:, :, 0, :, None] * PEER_NKEYS + top_i[:, :, 1, None, :]
    best_s, best_c = lax.top_k(cand.reshape(T, PEER_HEADS, PEER_TOPK * PEER_TOPK), PEER_TOPK)
    expert_idx = jnp.take_along_axis(
        cand_idx.reshape(T, PEER_HEADS, PEER_TOPK * PEER_TOPK), best_c, axis=-1)
    gates = jax.nn.softmax(best_s, axis=-1)
    nb = T // PEER_BLOCK

    def block_fn(args):
        xb, ib, gb = args
        u = jnp.take(expert_u, ib, axis=0)
        act = jax.nn.gelu(jnp.einsum('td,thkd->thk', xb, u).astype(jnp.float32))
        w = (gb * act).astype(xb.dtype)
        v = jnp.take(expert_v, ib, axis=0)
        return jnp.einsum('thk,thkd->td', w, v)

    out = lax.map(block_fn, (ht.reshape(nb, PEER_BLOCK, D),
                             expert_idx.reshape(nb, PEER_BLOCK, PEER_HEADS, PEER_TOPK),
                             gates.reshape(nb, PEER_BLOCK, PEER_HEADS, PEER_TOPK)))
    return out.reshape(B, S, D)


def setup_inputs(seed: int = 0) -> dict:
    key = jax.random.key(seed)
    ks = jax.random.split(key, 14)
    f32 = jnp.float32

    def nrm(k, shape, scale):
        return jax.random.normal(k, shape, f32) * scale

    return {
        "x": nrm(ks[0], (BATCH, SEQ, D_MODEL), 1.0),
        "norm_mix_w": 1.0 + nrm(ks[1], (DEPTH, D_MODEL), 0.02),
        "w_in": nrm(ks[2], (DEPTH, D_MODEL, IN_WIDTH), D_MODEL ** -0.5),
        "hg_lower_bounds": nrm(ks[3], (DEPTH + 1, HG_HEADS * HG_DK), 0.1),
        "hg_norm_w": 1.0 + nrm(ks[4], (DEPTH, HG_HEADS * HG_DV), 0.02),
        "w_branch_hg": nrm(ks[5], (DEPTH, HG_HEADS * HG_DV, D_MODEL), (HG_HEADS * HG_DV) ** -0.5),
        "w_branch_ret": nrm(ks[6], (DEPTH, RET_HEADS * RET_DV, D_MODEL), (RET_HEADS * RET_DV) ** -0.5),
        "w_out": nrm(ks[7], (DEPTH, D_MODEL, D_MODEL), D_MODEL ** -0.5),
        "norm_ffn_w": 1.0 + nrm(ks[8], (DEPTH, D_MODEL), 0.02),
        "peer_w_q": nrm(ks[9], (DEPTH, D_MODEL, PEER_HEADS * PEER_QDIM), D_MODEL ** -0.5),
        "peer_sub_keys": nrm(ks[10], (DEPTH, PEER_HEADS, 2, PEER_NKEYS, PEER_HALF), PEER_HALF ** -0.5),
        "expert_u": nrm(ks[11], (DEPTH, PEER_N_EXPERTS, D_MODEL), D_MODEL ** -0.5),
        "expert_v": nrm(ks[12], (DEPTH, PEER_N_EXPERTS, D_MODEL), PEER_HEADS ** -0.5),
        "final_norm_w": 1.0 + nrm(ks[13], (D_MODEL,), 0.02),
    }


def reference(x, norm_mix_w, w_in, hg_lower_bounds, hg_norm_w, w_branch_hg, w_branch_ret,
              w_out, norm_ffn_w, peer_w_q, peer_sub_keys, expert_u, expert_v, final_norm_w):
    B, S, _ = x.shape
    f32 = jnp.float32
    lbs = jnp.cumsum(jax.nn.softmax(hg_lower_bounds.astype(f32), axis=0), axis=0)
    pos = jnp.arange(S, dtype=f32)
    for l in range(DEPTH):
        h = rmsnorm(x, norm_mix_w[l])
        proj = h @ w_in[l]
        hq, hf, hi, hg, rq, rk, rv, rg, gate_a, gate_b = split_cols(proj, IN_SIZES)

        o_hg = hgrn2_mix(hq.astype(f32), hf.astype(f32), hi.astype(f32), lbs[l])
        o_hg = (head_rms(o_hg).reshape(B, S, HG_HEADS * HG_DV) * hg_norm_w[l].astype(f32)
                * jax.nn.silu(hg.astype(f32)))

        q = rotary(rq.astype(f32).reshape(B, S, RET_HEADS, RET_DK), pos)
        k = rotary(rk.astype(f32).reshape(B, S, RET_HEADS, RET_DK), pos) * RET_DK ** -0.5
        v = rv.astype(f32).reshape(B, S, RET_HEADS, RET_DV)
        o_ret = retention_mix(q, k, v)
        o_ret = head_rms(o_ret).reshape(B, S, RET_HEADS * RET_DV) * jax.nn.silu(rg.astype(f32))

        y_hg = o_hg.astype(x.dtype) @ w_branch_hg[l]
        y_ret = o_ret.astype(x.dtype) @ w_branch_ret[l]
        merged = jax.nn.sigmoid(gate_a) * y_hg + jax.nn.sigmoid(gate_b) * y_ret
        x = x + merged @ w_out[l]

        h = rmsnorm(x, norm_ffn_w[l])
        x = x + peer_ffn(h, peer_w_q[l], peer_sub_keys[l], expert_u[l], expert_v[l])
    return rmsnorm(x, final_norm_w)
```

```python
import numpy as np
import ml_dtypes
from contextlib import ExitStack
import concourse.bass as bass
import concourse.mybir as mybir
from concourse.bass_utils import run_bass_kernel_spmd

F32 = mybir.dt.float32
BF = mybir.dt.bfloat16
U32 = mybir.dt.uint32
AF = mybir.ActivationFunctionType
OP = mybir.AluOpType
AX = mybir.AxisListType

D = 1024
SEQ = 8192
NCORE = 8
EPS = 1e-6
NT1_FULL = SEQ // 128
NT2_FULL = 16


class Buf:
    __slots__ = ("w", "r", "excl")

    def __init__(self, excl=False):
        self.w = None
        self.r = {}
        self.excl = excl


class Sched:
    ENG = ("pe", "act", "dve", "pool", "sp")

    def __init__(self, nc, es, ndma=48):
        self.nc = nc
        self.sem = {e: es.enter_context(nc.semaphore("s_" + e)) for e in self.ENG}
        self.cnt = {e: 0 for e in self.ENG}
        self.prog = {e: [] for e in self.ENG}
        self.waited = {e: {} for e in self.ENG}
        self.dsem = [es.enter_context(nc.semaphore("d%d" % i)) for i in range(ndma + 1)]
        self.dcnt = [0] * (ndma + 1)
        self.dnext = 0
        self.ndma = ndma
        self.npool0 = 32
        self.pnext = 0

    def _semof(self, key):
        return self.sem[key] if isinstance(key, str) else self.dsem[key]

    def _wait(self, e, key, val):
        if self.waited[e].get(key, 0) >= val:
            return
        self.waited[e][key] = val
        self.prog[e].append(("w", self._semof(key), val))

    def _need(self, e, tok):
        k, v = tok
        if k == e and e in ("pe", "sp"):
            return
        self._wait(e, k, v)

    def _deps(self, e, reads, writes):
        for b in reads:
            if b.w is not None:
                self._need(e, b.w)
        for b in writes:
            if b.w is not None:
                self._need(e, b.w)
            for k, v in b.r.items():
                self._need(e, (k, v))

    def _mark(self, tok, reads, writes):
        k, v = tok
        for b in reads:
            if b.r.get(k, 0) < v:
                b.r[k] = v
        for b in writes:
            b.w = tok
            b.r = {}

    def op(self, e, fn, reads=(), writes=()):
        if any(b.excl for b in reads):
            writes = list(writes) + [b for b in reads if b.excl and b not in writes]
            reads = [b for b in reads if not b.excl]
        self._deps(e, reads, writes)
        self.cnt[e] += 1
        self.prog[e].append(("i", fn, self.sem[e], 1))
        self._mark((e, self.cnt[e]), reads, writes)

    def dma(self, q, fn, reads=(), writes=(), inc=16):
        self._deps(q, reads, writes)
        if q == "pool":
            i = self.npool0 + self.pnext
            self.pnext = (self.pnext + 1) % (self.ndma - self.npool0)
        else:
            i = self.dnext
            self.dnext = (self.dnext + 1) % self.npool0
        if self.dcnt[i] > 0:
            self._wait(q, i, self.dcnt[i])
        self.dcnt[i] += inc
        self.prog[q].append(("i", fn, self.dsem[i], inc))
        self._mark((i, self.dcnt[i]), reads, writes)

    def coll(self, fn, reads=(), writes=()):
        self._deps("pool", reads, writes)
        i = len(self.dsem) - 1
        self.dcnt[i] += 1
        self.prog["pool"].append(("i", fn, self.dsem[i], 1))
        self._mark((i, self.dcnt[i]), reads, writes)

    def barrier(self):
        for e in self.ENG:
            for k in self.ENG:
                if k != e and self.cnt[k] > 0:
                    self._wait(e, k, self.cnt[k])
            for i, c in enumerate(self.dcnt):
                if c > 0:
                    self._wait(e, i, c)

    def finish(self):
        for i, c in enumerate(self.dcnt):
            if c > 0:
                self._wait("sp", i, c)
        for k in self.ENG:
            if k != "sp" and self.cnt[k] > 0:
                self._wait("sp", k, self.cnt[k])

    def emit(self, block):
        def mk(e):
            def body(eng):
                for it in self.prog[e]:
                    if it[0] == "w":
                        eng.wait_ge(it[1], it[2])
                    else:
                        it[1](eng).then_inc(it[2], it[3])
            return body
        block.tensor(mk("pe"))
        block.scalar(mk("act"))
        block.vector(mk("dve"))
        block.gpsimd(mk("pool"))
        block.sync(mk("sp"))


class Arena:
    def __init__(self, t, n):
        self.t = t
        self.n = n
        self.off = 0
        self.mark = 0

    def get(self, n):
        assert self.off + n <= self.n, ("arena overflow", self.off, n, self.n)
        ap = self.t[:, self.off:self.off + n]
        self.off += n
        return ap


class K:
    def __init__(self, S):
        self.S = S

    def mm(self, out, lhsT, rhs, start, stop, r=(), w=()):
        self.S.op("pe", lambda e: e.matmul(out, lhsT, rhs, start=start, stop=stop), r, w)

    def act(self, out, in_, func, r=(), w=(), bias=0.0, scale=1.0, accum_out=None):
        if accum_out is None:
            self.S.op("act", lambda e: e.activation(out, in_, func, bias=bias, scale=scale), r, w)
        else:
            self.S.op("act", lambda e: e.activation(out, in_, func, bias=bias, scale=scale,
                                                    accum_out=accum_out), r, w)

    def ts(self, eng, out, in0, s1, s2, op0, op1=None, r=(), w=()):
        if op1 is None:
            self.S.op(eng, lambda e: e.tensor_scalar(out, in0, s1, None, op0), r, w)
        else:
            self.S.op(eng, lambda e: e.tensor_scalar(out, in0, s1, s2, op0, op1), r, w)

    def tt(self, eng, out, in0, in1, op, r=(), w=()):
        self.S.op(eng, lambda e: e.tensor_tensor(out, in0, in1, op), r, w)

    def stt(self, out, in0, scalar, in1, op0, op1, r=(), w=(), accum_out=None):
        if accum_out is None:
            self.S.op("dve", lambda e: e.scalar_tensor_tensor(out, in0, scalar, in1, op0, op1), r, w)
        else:
            self.S.op("dve", lambda e: e.scalar_tensor_tensor(out, in0, scalar, in1, op0, op1,
                                                              accum_out=accum_out), r, w)

    def cp(self, eng, out, in_, r=(), w=()):
        self.S.op(eng, lambda e: e.tensor_copy(out, in_), r, w)

    def recip(self, out, in_, r=(), w=()):
        self.S.op("dve", lambda e: e.reciprocal(out, in_), r, w)

    def ld(self, q, out, in_, r=(), w=(), slow=False):
        if slow:
            self.S.dma(q, lambda e: e.dma_start(out=out, in_=in_, allow_slow_non_contiguous=True), r, w)
        else:
            self.S.dma(q, lambda e: e.dma_start(out=out, in_=in_), r, w)


def load_w(k, stg, b_stg, dst, b_dst, src, nk, ncol, ci0=0):
    srcv = src.rearrange("(k p) n -> p k n", p=128)
    ci = ci0
    step = 1024
    for kk in range(nk):
        for c0 in range(0, ncol, step):
            s = ci % 2
            k.ld("sp", stg[s][:, 0:step], srcv[:, kk, c0:c0 + step], w=[b_stg[s]])
            eng = ("dve", "pool", "act")[ci % 3]
            d = dst[:, kk, c0:c0 + step]
            if eng == "act":
                k.act(d, stg[s][:, 0:step], AF.Copy, r=[b_stg[s]], w=[b_dst])
            else:
                k.cp(eng, d, stg[s][:, 0:step], r=[b_stg[s]], w=[b_dst])
            ci += 1
    return ci


def phase2(nc, es, S, k, a16, a32, PS, PB, ident, b_const, obuf, b_obuf, gath, x1buf, xo, oidx_d,
           wg_d, wbh_d, wbr_d, wo_d, nmw_t, nfw_d, fnw_d, wq_d, skT_d, eu_d, ev_d, iota_d, yout, nt2, mock):
    S.barrier()
    b_gath = Buf()
    if not mock:
        S.coll(lambda e: e.collective_compute("AllGather", OP.bypass, replica_groups=[list(range(NCORE))],
                                              ins=[obuf[:, :]], outs=[gath[:, :]]), [b_obuf], [b_gath])
    a16.off, a32.off = a16.mark, a32.mark
    oidx = es.enter_context(nc.sbuf_tensor("oidx_sb", [128, 64], U32))
    tiu = es.enter_context(nc.sbuf_tensor("tiu", [128, 256], U32))
    eidx = es.enter_context(nc.sbuf_tensor("eidx", [128, 128], U32))
    b_oidx = Buf()
    k.ld("sp", oidx[:, :], oidx_d[:, :], w=[b_oidx])

    m16, m32 = a16.off, a32.off
    wg = a16.get(8 * 2048).rearrange("p (k n) -> p k n", k=8)
    wbh = a16.get(8 * 1024).rearrange("p (k n) -> p k n", k=8)
    wbr = a16.get(16 * 1024).rearrange("p (k n) -> p k n", k=16)
    wo = a16.get(8 * 1024).rearrange("p (k n) -> p k n", k=8)
    b_w2 = Buf()
    stg = [a32.get(1024) for _ in range(2)]
    b_stg = [Buf(), Buf()]
    ci = load_w(k, stg, b_stg, wg, b_w2, wg_d, 8, 2048)
    ci = load_w(k, stg, b_stg, wbh, b_w2, wbh_d, 8, 1024, ci)
    ci = load_w(k, stg, b_stg, wbr, b_w2, wbr_d, 16, 1024, ci)
    ci = load_w(k, stg, b_stg, wo, b_w2, wo_d, 8, 1024, ci)
    xt = [a32.get(1024) for _ in range(2)]
    b_xt = [Buf(), Buf()]
    sqj = a16.get(1024)
    xn = a16.get(1024)
    hT = a16.get(1024).rearrange("p (k t) -> p k t", k=8)
    b_sq, b_xn, b_hT = Buf(), Buf(), Buf()
    stat = a32.get(16)
    b_stat = Buf()
    sgate = a32.get(2048)
    b_sgate = Buf()
    ot = a16.get(3072).rearrange("p (r f) -> p r f", r=4)
    b_ot = Buf()
    oT = a16.get(3072).rearrange("p (c t) -> p c t", c=24)
    b_oT = Buf()
    m1 = a32.get(1024)
    b_m1 = Buf()
    mbf = a16.get(1024)
    b_mbf = Buf()
    mT = a16.get(1024).rearrange("p (k t) -> p k t", k=8)
    b_mT = Buf()
    x1 = a32.get(1024)
    b_x1 = Buf()
    b_x1buf = Buf()

    def norm_T(src, b_src, use_nmw):
        ss, sq_, rstd = stat[:, 0:1], stat[:, 1:2], stat[:, 2:3]
        k.act(sqj, src, AF.Square, r=[b_src], w=[b_sq, b_stat], accum_out=ss)
        k.act(sq_, ss, AF.Sqrt, r=[b_stat], w=[b_stat], bias=EPS, scale=1.0 / D)
        k.recip(rstd, sq_, r=[b_stat], w=[b_stat])
        k.ts("dve", xn, src, rstd, None, OP.mult, r=[b_src, b_stat], w=[b_xn])
        for c in range(8):
            pb = c // 4
            k.mm(PS[pb][:, (c % 4) * 128:(c % 4 + 1) * 128], xn[:, c * 128:(c + 1) * 128], ident,
                 True, True, r=[b_xn, b_const], w=[PB[pb]])
        for c in range(8):
            pb = c // 4
            srcp = PS[pb][:, (c % 4) * 128:(c % 4 + 1) * 128]
            k.ts("dve", hT[:, c, :], srcp, use_nmw[:, c:c + 1], None, OP.mult, r=[PB[pb], b_const], w=[b_hT])

    for i in range(nt2):
        s = i % 2
        k.ld("sp", xt[s], xo[i * 128:(i + 1) * 128, :], w=[b_xt[s]])
        for r in range(4):
            src_rows = gath[:, :]
            if mock:
                k.ld("sp", ot[:, r, :], obuf[i * 128:(i + 1) * 128, :], r=[b_gath, b_obuf], w=[b_ot])
            else:
                S.dma("pool", (lambda o_, c_: (lambda e: e.indirect_dma_start(
                    o_, None, src_rows, bass.IndirectOffsetOnAxis(ap=oidx[:, c_:c_ + 1], axis=0))))(ot[:, r, :], i * 4 + r),
                    [b_gath, b_oidx], [b_ot])
        norm_T(xt[s], b_xt[s], nmw_t)
        for j in range(4):
            pb = 2 + j
            for kk in range(8):
                k.mm(PS[pb][:, :], hT[:, kk, :], wg[:, kk, j * 512:(j + 1) * 512], kk == 0, kk == 7,
                     r=[b_hT, b_w2], w=[PB[pb]])
            k.act(sgate[:, j * 512:(j + 1) * 512], PS[pb][:, :], AF.Sigmoid, r=[PB[pb]], w=[b_sgate])
        for c in range(24):
            pb = 6 + (c // 4) % 2
            r_, f_ = divmod(c, 6)
            k.mm(PS[pb][:, (c % 4) * 128:(c % 4 + 1) * 128], ot[:, r_, f_ * 128:(f_ + 1) * 128], ident,
                 True, True, r=[b_ot, b_const], w=[PB[pb]])
            if c % 4 == 3:
                c0 = c - 3
                k.cp("dve", oT[:, c0:c0 + 4, :], PS[pb][:, :].rearrange("p (c t) -> p c t", c=4),
                     r=[PB[pb]], w=[b_oT])
        for half in range(2):
            n = 0
            for r_ in range(4):
                for j in range(2):
                    k.mm(PS[half][:, :], oT[:, r_ * 6 + j, :], wbh[:, 2 * r_ + j, half * 512:(half + 1) * 512],
                         n == 0, n == 7, r=[b_oT, b_w2], w=[PB[half]])
                    n += 1
            n = 0
            for r_ in range(4):
                for m in range(4):
                    k.mm(PS[2 + half][:, :], oT[:, r_ * 6 + 2 + m, :], wbr[:, r_ * 4 + m, half * 512:(half + 1) * 512],
                         n == 0, n == 15, r=[b_oT, b_w2], w=[PB[2 + half]])
                    n += 1
        for half in range(2):
            cs = slice(half * 512, (half + 1) * 512)
            k.tt("dve", m1[:, cs], PS[half][:, :], sgate[:, half * 512:(half + 1) * 512], OP.mult,
                 r=[PB[half], b_sgate], w=[b_m1])
            k.tt("dve", mbf[:, cs], PS[2 + half][:, :], sgate[:, 1024 + half * 512:1024 + (half + 1) * 512], OP.mult,
                 r=[PB[2 + half], b_sgate], w=[b_mbf])
            k.tt("dve", mbf[:, cs], mbf[:, cs], m1[:, cs], OP.add, r=[b_mbf, b_m1], w=[b_mbf])
        for c in range(8):
            pb = 4 + c // 4
            k.mm(PS[pb][:, (c % 4) * 128:(c % 4 + 1) * 128], mbf[:, c * 128:(c + 1) * 128], ident, True, True,
                 r=[b_mbf, b_const], w=[PB[pb]])
            if c % 4 == 3:
                c0 = c - 3
                k.cp("dve", mT[:, c0:c0 + 4, :], PS[pb][:, :].rearrange("p (c t) -> p c t", c=4),
                     r=[PB[pb]], w=[b_mT])
        for half in range(2):
            pb = 6 + half
            for kk in range(8):
                k.mm(PS[pb][:, :], mT[:, kk, :], wo[:, kk, half * 512:(half + 1) * 512], kk == 0, kk == 7,
                     r=[b_mT, b_w2], w=[PB[pb]])
            k.tt("dve", x1[:, half * 512:(half + 1) * 512], PS[pb][:, :], xt[s][:, half * 512:(half + 1) * 512], OP.add,
                 r=[PB[pb], b_xt[s]], w=[b_x1])
        k.ld("sp", x1buf[i * 128:(i + 1) * 128, :], x1, r=[b_x1], w=[b_x1buf])

    S.barrier()
    a16.off, a32.off = m16, m32
    wq = a16.get(8 * 2048).rearrange("p (k n) -> p k n", k=8)
    skT = a16.get(2048).rearrange("p (g n) -> p g n", g=16)
    b_w3 = Buf()
    stg = [a32.get(1024) for _ in range(2)]
    b_stg = [Buf(), Buf()]
    load_w(k, stg, b_stg, wq, b_w3, wq_d, 8, 2048)
    for hh in range(2):
        k.ld("sp", stg[hh], skT_d[:, hh * 1024:(hh + 1) * 1024], w=[b_stg[hh]])
        k.cp("dve", skT[:, hh * 8:(hh + 1) * 8, :], stg[hh].rearrange("p (g n) -> p g n", g=8), r=[b_stg[hh]], w=[b_w3])
    nfw_bc = a32.get(1024)
    fnw_bc = a32.get(1024)
    iota = a32.get(128)
    b_c3 = Buf()
    k.ld("sp", nfw_bc, nfw_d[0:1, :].partition_broadcast(128), w=[b_c3])
    k.ld("sp", fnw_bc, fnw_d[0:1, :].partition_broadcast(128), w=[b_c3])
    k.ld("sp", iota, iota_d[:, :], w=[b_c3])
    x1t = a32.get(1024)
    b_x1t = Buf()
    h2 = a32.get(1024)
    b_h2 = Buf()
    hbf = a16.get(1024)
    b_hbf = Buf()
    hT = a16.get(1024).rearrange("p (k t) -> p k t", k=8)
    b_hT = Buf()
    qT = a16.get(2048).rearrange("p (g t) -> p g t", g=16)
    b_qT = Buf()
    junk = a16.get(1024)
    b_junk = Buf()
    stat = a32.get(32)
    b_stat = Buf()
    sc = a32.get(2048).rearrange("p (g n) -> p g n", g=16)
    b_sc = Buf()
    scr = a32.get(256)
    b_scr = Buf()
    tv = a32.get(256).rearrange("p (g n) -> p g n", g=16)
    tif = a32.get(256).rearrange("p (g n) -> p g n", g=16)
    b_tv, b_ti, b_tif = Buf(), Buf(), Buf()
    cand = a32.get(256)
    cidx = a32.get(256)
    b_cand, b_cidx = Buf(), Buf()
    bv = a32.get(128).rearrange("p (h n) -> p h n", h=8)
    ex = a32.get(128).rearrange("p (h n) -> p h n", h=8)
    b_bv, b_ex = Buf(), Buf()
    ef = a32.get(128)
    b_ef = Buf()
    b_eidx = Buf()
    dots = a32.get(128)
    b_dots = Buf()
    g1 = a32.get(128)
    g2 = a32.get(128)
    wgt = a32.get(128)
    b_g, b_wgt = Buf(), Buf()
    NB = 4
    gbuf = es.enter_context(nc.sbuf_tensor("gbuf", [128, 4096], F32))
    ub = [a32.get(1024) for _ in range(2)] + [gbuf[:, 0:1024], gbuf[:, 1024:2048]]
    vb = [a32.get(1024) for _ in range(2)] + [gbuf[:, 2048:3072], gbuf[:, 3072:4096]]
    b_ub = [Buf() for _ in range(NB)]
    b_vb = [Buf() for _ in range(NB)]
    acc = a32.get(1024)
    b_acc = Buf()
    b_y = Buf()
    vbf = [a16.get(1024) for _ in range(2)]
    b_vbf = [Buf(), Buf()]
    dg = [a16.get(128) for _ in range(2)]
    b_dg = [Buf(), Buf()]
    tvu = tiu[:, :].rearrange("p (g n) -> p g n", g=16)
    fz = a32.get(8)
    b_fz = Buf()
    S.op("pool", (lambda ap: (lambda e: e.memset(ap, 0.0)))(fz), (), [b_fz])
    NEG = -3.0e38

    for i in range(nt2):
        k.ld("sp", x1t, x1buf[i * 128:(i + 1) * 128, :], r=[b_x1buf], w=[b_x1t])
        ss, sq_, rstd = stat[:, 0:1], stat[:, 1:2], stat[:, 2:3]
        k.act(junk, x1t, AF.Square, r=[b_x1t], w=[b_junk, b_stat], accum_out=ss)
        k.act(sq_, ss, AF.Sqrt, r=[b_stat], w=[b_stat], bias=EPS, scale=1.0 / D)
        k.recip(rstd, sq_, r=[b_stat], w=[b_stat])
        k.stt(h2, x1t, rstd, nfw_bc, OP.mult, OP.mult, r=[b_x1t, b_stat, b_c3], w=[b_h2])
        k.cp("dve", hbf, h2, r=[b_h2], w=[b_hbf])
        for c in range(8):
            pb = c // 4
            k.mm(PS[pb][:, (c % 4) * 128:(c % 4 + 1) * 128], hbf[:, c * 128:(c + 1) * 128], ident,
                 True, True, r=[b_hbf, b_const], w=[PB[pb]])
            if c % 4 == 3:
                c0 = c - 3
                k.cp("dve", hT[:, c0:c0 + 4, :], PS[pb][:, :].rearrange("p (c t) -> p c t", c=4),
                     r=[PB[pb]], w=[b_hT])
        for gq in range(16):
            pb = 2 + gq // 4
            for kk in range(8):
                k.mm(PS[pb][:, (gq % 4) * 128:(gq % 4 + 1) * 128], wq[:, kk, gq * 128:(gq + 1) * 128], hT[:, kk, :],
                     kk == 0, kk == 7, r=[b_w3, b_hT], w=[PB[pb]])
            if gq % 4 == 3:
                g0 = gq - 3
                k.cp("dve", qT[:, g0:g0 + 4, :], PS[pb][:, :].rearrange("p (c t) -> p c t", c=4),
                     r=[PB[pb]], w=[b_qT])
        for gq in range(16):
            pb = 6 + (gq // 4) % 2
            k.mm(PS[pb][:, (gq % 4) * 128:(gq % 4 + 1) * 128], qT[:, gq, :], skT[:, gq, :], True, True,
                 r=[b_qT, b_w3], w=[PB[pb]])
            if gq % 4 == 3:
                g0 = gq - 3
                k.cp("dve", sc[:, g0:g0 + 4, :], PS[pb][:, :].rearrange("p (c t) -> p c t", c=4),
                     r=[PB[pb]], w=[b_sc])
        for gq in range(16):
            S.op("dve", (lambda o_, i_: (lambda e: e.max(o_, i_)))(tv[:, gq, 0:8], sc[:, gq, :]), [b_sc], [b_tv])
            S.op("dve", (lambda o_, m_, i_: (lambda e: e.max_index(o_, m_, i_)))(tvu[:, gq, 0:8], tv[:, gq, 0:8], sc[:, gq, :]),
                 [b_sc, b_tv], [b_ti])
            S.op("dve", (lambda o_, m_, i_: (lambda e: e.match_replace(o_, m_, i_, NEG)))(scr[:, 0:128], tv[:, gq, 0:8], sc[:, gq, :]),
                 [b_sc, b_tv], [b_scr])
            S.op("dve", (lambda o_, i_: (lambda e: e.max(o_, i_)))(tv[:, gq, 8:16], scr[:, 0:128]), [b_scr], [b_tv])
            S.op("dve", (lambda o_, m_, i_: (lambda e: e.max_index(o_, m_, i_)))(tvu[:, gq, 8:16], tv[:, gq, 8:16], scr[:, 0:128]),
                 [b_scr, b_tv], [b_ti])
        k.cp("dve", tif[:, :, :], tvu[:, :, :], r=[b_ti], w=[b_tif])
        for h in range(8):
            c3 = cand.rearrange("p (a b) -> p a b", a=16)
            x3 = cidx.rearrange("p (a b) -> p a b", a=16)
            A0 = tv[:, 2 * h, :].unsqueeze(2).to_broadcast([128, 16, 16])
            B0 = tv[:, 2 * h + 1, :].unsqueeze(1).to_broadcast([128, 16, 16])
            k.tt("dve", c3, A0, B0, OP.add, r=[b_tv], w=[b_cand])
            k.ts("dve", x3, tif[:, 2 * h, :].unsqueeze(2).to_broadcast([128, 16, 16]), 128.0, None, OP.mult,
                 r=[b_tif], w=[b_cidx])
            k.tt("dve", x3, x3, tif[:, 2 * h + 1, :].unsqueeze(1).to_broadcast([128, 16, 16]), OP.add,
                 r=[b_cidx, b_tif], w=[b_cidx])
            S.op("dve", (lambda o_, i_: (lambda e: e.max(o_, i_)))(bv[:, h, 0:8], cand), [b_cand], [b_bv])
            S.op("dve", (lambda o_, m_, i_: (lambda e: e.match_replace(o_, m_, i_, NEG)))(scr, bv[:, h, 0:8], cand),
                 [b_cand, b_bv], [b_scr])
            S.op("dve", (lambda o_, i_: (lambda e: e.max(o_, i_)))(bv[:, h, 8:16], scr), [b_scr], [b_bv])
            for kq in range(16):
                slot = h * 16 + kq
                k.stt(scr, cand, bv[:, h, kq:kq + 1], cidx, OP.is_equal, OP.mult,
                      r=[b_cand, b_bv, b_cidx], w=[b_scr, b_ef], accum_out=ef[:, slot:slot + 1])
        k.ts("dve", fz, fz, 1.0, None, OP.mult, r=[b_fz], w=[b_fz, b_ef])
        k.ts("dve", ef, ef, 0.0, 16383.0, OP.max, OP.min, r=[b_ef], w=[b_ef])
        k.cp("dve", eidx[:, :], ef, r=[b_ef], w=[b_eidx])
        negm = stat[:, 8:16]
        k.ts("dve", negm, bv[:, :, 0], -1.0, None, OP.mult, r=[b_bv], w=[b_stat])
        for h in range(8):
            k.act(ex[:, h, :], bv[:, h, :], AF.Exp, r=[b_bv, b_stat], w=[b_ex, b_stat], bias=negm[:, h:h + 1],
                  accum_out=stat[:, 16 + h:17 + h])
        k.recip(stat[:, 24:32], stat[:, 16:24], r=[b_stat], w=[b_stat])
        k.tt("dve", ex[:, :, :], ex[:, :, :], stat[:, 24:32].unsqueeze(2).to_broadcast([128, 8, 16]), OP.mult,
             r=[b_ex, b_stat], w=[b_ex])
        for slot in range(128):
            bi = slot % NB
            S.dma("pool", (lambda o_, c_: (lambda e: e.indirect_dma_start(
                o_, None, eu_d[:, :], bass.IndirectOffsetOnAxis(ap=eidx[:, c_:c_ + 1], axis=0))))(ub[bi], slot),
                [b_eidx], [b_ub[bi]])
            k.stt(junk, ub[bi], 1.0, h2, OP.mult, OP.mult, r=[b_ub[bi], b_h2], w=[b_junk, b_dots],
                  accum_out=dots[:, slot:slot + 1])
        k.ts("dve", fz, fz, 1.0, None, OP.mult, r=[b_fz], w=[b_fz, b_dots])
        k.tt("dve", g1, dots, dots, OP.mult, r=[b_dots], w=[b_g])
        k.ts("dve", g1, g1, 0.044715, 1.0, OP.mult, OP.add, r=[b_g], w=[b_g])
        k.tt("dve", g1, g1, dots, OP.mult, r=[b_g, b_dots], w=[b_g])
        k.act(g2, g1, AF.Sigmoid, r=[b_g], w=[b_g], scale=1.5957691216057308)
        k.tt("dve", g2, g2, dots, OP.mult, r=[b_g, b_dots], w=[b_g])
        k.tt("dve", wgt, g2, ex[:, :, :].rearrange("p h n -> p (h n)"), OP.mult, r=[b_g, b_ex], w=[b_wgt])
        for slot in range(128):
            bi = slot % NB
            S.dma("pool", (lambda o_, c_: (lambda e: e.indirect_dma_start(
                o_, None, ev_d[:, :], bass.IndirectOffsetOnAxis(ap=eidx[:, c_:c_ + 1], axis=0))))(vb[bi], slot),
                [b_eidx], [b_vb[bi]])
            vs = slot % 2
            k.act(vbf[vs], vb[bi], AF.Copy, r=[b_vb[bi]], w=[b_vbf[vs]])
            k.act(dg[vs], ident, AF.Copy, r=[b_const, b_wgt], w=[b_dg[vs]], scale=wgt[:, slot:slot + 1])
            for half in range(2):
                k.mm(PS[4 + half][:, :], dg[vs], vbf[vs][:, half * 512:(half + 1) * 512], slot == 0, slot == 127,
                     r=[b_dg[vs], b_vbf[vs]], w=[PB[4 + half]])
        for half in range(2):
            cs = slice(half * 512, (half + 1) * 512)
            k.tt("dve", acc[:, cs], PS[4 + half][:, :], x1t[:, cs], OP.add, r=[PB[4 + half], b_x1t], w=[b_acc])
        ss2, sq2, rs2 = stat[:, 3:4], stat[:, 4:5], stat[:, 5:6]
        k.act(junk, acc, AF.Square, r=[b_acc], w=[b_junk, b_stat], accum_out=ss2)
        k.act(sq2, ss2, AF.Sqrt, r=[b_stat], w=[b_stat], bias=EPS, scale=1.0 / D)
        k.recip(rs2, sq2, r=[b_stat], w=[b_stat])
        k.stt(acc, acc, rs2, fnw_bc, OP.mult, OP.mult, r=[b_acc, b_stat, b_c3], w=[b_acc])
        k.ld("sp", yout[i * 128:(i + 1) * 128, :], acc, r=[b_acc], w=[b_y])


def build_nc(nt1=NT1_FULL, nt2=NT2_FULL, stage=2, cut=0, mock=False):
    nc = bass.Bass("TRN2", target_bir_lowering=False)
    dt_in = {}

    def din(name, shape, dt=F32):
        t = nc.dram_tensor(name, list(shape), dt, kind="ExternalInput")
        dt_in[name] = t
        return t.ap()

    xb = din("xb", [nt1 * 128, D])
    w1 = din("w1", [D, 2816])
    nmw = din("nmw", [128, 8])
    lbr = din("lbr", [2, 256])
    lbc = din("lbc", [128, 4])
    hnw = din("hnw", [1, 256])
    ident_d = din("ident", [128, 128], BF)
    rot_d = din("rot", [128, nt1, 384])
    dmaskT_d = din("dmaskT", [128, 128])
    qdec_d = din("qdec", [128, 128])
    cols_d = din("cols", [128, 4])
    trimid_d = din("trimid", [128, 130], BF)
    causT_d = din("causT", [128, 128])

    obuf = nc.dram_tensor("obuf", [SEQ, 768], BF)
    if stage == 1:
        y1 = nc.dram_tensor("y1", [nt1 * 128, 768], BF, kind="ExternalOutput").ap()
    else:
        gath = nc.dram_tensor("gath", [8 * SEQ, 768], BF)
        x1buf = nc.dram_tensor("x1buf", [nt2 * 128, D], F32)
        xo = din("xo", [nt2 * 128, D])
        oidx_d = din("oidx", [128, 64], U32)
        wg_d = din("wg", [D, 2048])
        wbh_d = din("wbh", [D, D])
        wbr_d = din("wbr", [2048, D])
        wo_d = din("wo", [D, D])
        nfw_d = din("nfw", [1, D])
        fnw_d = din("fnw", [1, D])
        wq_d = din("wq", [D, 2048])
        skT_d = din("skT", [128, 2048])
        eu_d = din("eu", [16384, D])
        ev_d = din("ev", [16384, D])
        iota_d = din("iota", [128, 128])
        yout = nc.dram_tensor("y", [nt2 * 128, D], F32, kind="ExternalOutput").ap()

    es = ExitStack()
    with es:
        S = Sched(nc, es)
        k = K(S)
        a16 = Arena(es.enter_context(nc.sbuf_tensor("a16", [128, 61000], BF)), 61000)
        a32 = Arena(es.enter_context(nc.sbuf_tensor("a32", [128, 17200], F32)), 17200)
        PS = [es.enter_context(nc.psum_tensor("ps%d" % i, [128, 512], F32)) for i in range(8)]
        PB = [Buf(excl=True) for _ in range(8)]

        ident = a16.get(128)
        b_const = Buf()
        k.ld("sp", ident, ident_d[:, :], w=[b_const])
        nmw_t = a32.get(8)
        k.ld("sp", nmw_t, nmw[:, :], w=[b_const])

        a16.mark, a32.mark = a16.off, a32.off
        wreg = a16.get(8 * 2816).rearrange("p (k n) -> p k n", k=8)
        b_w = Buf()
        stg = [a32.get(1408) for _ in range(2)]
        b_stg = [Buf(), Buf()]
        w1v = w1.rearrange("(k p) n -> p k n", p=128)
        ci = 0
        for kk in range(8):
            for hf in range(2):
                s = ci % 2
                k.ld("sp", stg[s], w1v[:, kk, hf * 1408:(hf + 1) * 1408], w=[b_stg[s]])
                eng = ("dve", "pool", "act")[ci % 3]
                dst = wreg[:, kk, hf * 1408:(hf + 1) * 1408]
                if eng == "act":
                    k.act(dst, stg[s], AF.Copy, r=[b_stg[s]], w=[b_w])
                else:
                    k.cp(eng, dst, stg[s], r=[b_stg[s]], w=[b_w])
                ci += 1

        dmaskT = a32.get(128)
        qdec = a32.get(128)
        cols = a32.get(4)
        trimid = a16.get(130)
        g_hi = a16.get(256)
        g_lo = a16.get(256)
        b_ghl = Buf()
        causT = a32.get(128)
        lb_bc = a32.get(256)
        oml_bc = a32.get(256)
        lbtmp = a32.get(512).rearrange("p (a n) -> p a n", a=2)
        nw_bc = a32.get(256)
        lbc_t = a32.get(4)
        lbcol = a32.get(2)
        omlcol = a32.get(2)
        k.ld("sp", dmaskT, dmaskT_d[:, :], w=[b_const])
        k.ld("sp", qdec, qdec_d[:, :], w=[b_const])
        k.ld("sp", cols, cols_d[:, :], w=[b_const])
        k.ld("sp", trimid, trimid_d[:, :], w=[b_const])
        k.ld("sp", causT, causT_d[:, :], w=[b_const])
        k.ld("sp", lbc_t, lbc[:, :], w=[b_const])
        b_lb = Buf()
        for a in range(2):
            k.ld("sp", lbtmp[:, a, :], lbr[a:a + 1, :].partition_broadcast(128), w=[b_lb])
        k.ld("sp", nw_bc, hnw[0:1, :].partition_broadcast(128), w=[b_lb])
        k.tt("dve", lb_bc, lbtmp[:, 0, :], lbtmp[:, 1, :], OP.subtract, r=[b_lb], w=[b_lb])
        k.act(lb_bc, lb_bc, AF.Sigmoid, r=[b_lb], w=[b_lb])
        k.ts("dve", oml_bc, lb_bc, -1.0, 1.0, OP.mult, OP.add, r=[b_lb], w=[b_lb])
        k.tt("dve", lbcol, lbc_t[:, 0:2], lbc_t[:, 2:4], OP.subtract, r=[b_const], w=[b_lb])
        k.act(lbcol, lbcol, AF.Sigmoid, r=[b_lb], w=[b_lb])
        k.ts("dve", omlcol, lbcol, -1.0, 1.0, OP.mult, OP.add, r=[b_lb], w=[b_lb])

        xt = [a32.get(1024) for _ in range(2)]
        b_xt = [Buf(), Buf()]
        rot = [a32.get(384) for _ in range(2)]
        b_rot = [Buf(), Buf()]
        sqj = a16.get(1024)
        xn = a16.get(1024)
        hT = a16.get(1024).rearrange("p (k t) -> p k t", k=8)
        b_xn, b_hT, b_sq = Buf(), Buf(), Buf()
        stat = a32.get(16)
        b_stat = Buf()
        M1 = a32.get(512)
        M2 = a32.get(512)
        b_M = Buf()
        qk = a16.get(512).rearrange("p (h w t) -> p h w t", h=2, w=2)
        b_qk = Buf()
        At = a16.get(128)
        b_At = Buf()
        kdec = a16.get(256)
        b_kdec = Buf()
        v_r = a16.get(512)
        b_vr = Buf()
        qdT = a16.get(256).rearrange("p (h t) -> p h t", h=2)
        b_qdT = Buf()
        Sr = [a32.get(512) for _ in range(2)]
        Sbf = [a16.get(512) for _ in range(2)]
        b_Sr = [Buf(), Buf()]
        b_Sbf = [Buf(), Buf()]
        sig = a32.get(512)
        sg = a32.get(512)
        b_sig, b_sg = Buf(), Buf()
        sig_tm = a32.get(256)
        g_tm = a32.get(256)
        sgn = a32.get(256)
        b_tm, b_g, b_sgn = Buf(), Buf(), Buf()
        EB = a32.get(256).rearrange("p (j t) -> p j t", j=2)
        ENB = a32.get(260).rearrange("p (j t) -> p j t", j=2)
        b_EB, b_ENB = Buf(), Buf()
        qdTh = a16.get(256).rearrange("p (j t) -> p j t", j=2)
        kdTh = a16.get(256).rearrange("p (j t) -> p j t", j=2)
        b_qdTh, b_kdTh = Buf(), Buf()
        Ath = a16.get(256).rearrange("p (j t) -> p j t", j=2)
        b_Ath = Buf()
        vh = a16.get(256)
        b_vh = Buf()
        kdt = a16.get(256)
        b_kdt = Buf()
        Sh = a32.get(256).rearrange("p (j t) -> p j t", j=2)
        b_Sh = [Buf(), Buf()]
        Spb = a16.get(256).rearrange("p (j t) -> p j t", j=2)
        b_Spb = [Buf(), Buf()]
        tmpS = a32.get(256).rearrange("p (j t) -> p j t", j=2)
        b_tmpS = [Buf(), Buf()]
        sigh = a32.get(256)
        ghw = a32.get(256)
        b_sigh, b_ghw = Buf(), Buf()
        ost = [a16.get(768) for _ in range(2)]
        b_ost = [Buf(), Buf()]
        b_obuf = Buf()

        for h in range(2):
            S.op("pool", (lambda ap: (lambda e: e.memset(ap, 0.0)))(Sr[h]), (), [b_Sr[h]])
            S.op("pool", (lambda ap: (lambda e: e.memset(ap, 0.0)))(Sbf[h]), (), [b_Sbf[h]])
            S.op("pool", (lambda ap: (lambda e: e.memset(ap, 0.0)))(Sh[:, h, :]), (), [b_Sh[h]])

        def load_tile(i):
            s = i % 2
            k.ld("sp", xt[s], xb[i * 128:(i + 1) * 128, :], w=[b_xt[s]])
            k.ld("sp", rot[s], rot_d[:, i, :], w=[b_rot[s]])

        load_tile(0)
        for i in range(nt1):
            s = i % 2
            if i + 1 < nt1:
                load_tile(i + 1)
            ss, sq_, rstd = stat[:, 0:1], stat[:, 1:2], stat[:, 2:3]
            k.act(sqj, xt[s], AF.Square, r=[b_xt[s]], w=[b_sq, b_stat], accum_out=ss)
            k.act(sq_, ss, AF.Sqrt, r=[b_stat], w=[b_stat], bias=EPS, scale=1.0 / D)
            k.recip(rstd, sq_, r=[b_stat], w=[b_stat])
            k.ts("dve", xn, xt[s], rstd, None, OP.mult, r=[b_xt[s], b_stat], w=[b_xn])
            if cut == 1:
                break
            for c in range(8):
                pb = c // 4
                k.mm(PS[pb][:, (c % 4) * 128:(c % 4 + 1) * 128], xn[:, c * 128:(c + 1) * 128], ident,
                     True, True, r=[b_xn, b_const], w=[PB[pb]])
            for c in range(8):
                pb = c // 4
                src = PS[pb][:, (c % 4) * 128:(c % 4 + 1) * 128]
                if c % 2 == 0:
                    k.act(hT[:, c, :], src, AF.Copy, r=[PB[pb], b_const], w=[b_hT], scale=nmw_t[:, c:c + 1])
                else:
                    k.ts("dve", hT[:, c, :], src, nmw_t[:, c:c + 1], None, OP.mult,
                         r=[PB[pb], b_const], w=[b_hT])
            if cut == 2:
                break
            for j in range(8):
                pb = 2 + j // 4
                for kk in range(8):
                    k.mm(PS[pb][:, (j % 4) * 128:(j % 4 + 1) * 128], wreg[:, kk, j * 128:(j + 1) * 128],
                         hT[:, kk, :], kk == 0, kk == 7, r=[b_w, b_hT], w=[PB[pb]])
            for j, (pb, c0, n) in enumerate(((4, 1024, 512), (5, 1536, 512), (6, 2048, 512), (7, 2560, 256))):
                for kk in range(8):
                    k.mm(PS[pb][:, 0:n], hT[:, kk, :], wreg[:, kk, c0:c0 + n], kk == 0, kk == 7,
                         r=[b_w, b_hT], w=[PB[pb]])

            if cut == 3:
                break
            rs = rot[s].rearrange("p (a t) -> p a t", a=3)
            F2 = PS[3][:, :].rearrange("p (w h t) -> p w h t", w=2, h=2)
            M1v = M1.rearrange("p (w h t) -> p w h t", w=2, h=2)
            M2v = M2.rearrange("p (w h t) -> p w h t", w=2, h=2)
            for wv in range(2):
                k.tt("dve", M1v[:, wv, :, :], F2[:, wv, :, :], rs[:, 0:2, :], OP.mult,
                     r=[PB[3], b_rot[s]], w=[b_M])
                k.tt("dve", M2v[:, wv, :, :], F2[:, wv, :, :], rs[:, 1:3, :], OP.mult,
                     r=[PB[3], b_rot[s]], w=[b_M])
            k.tt("pool", qk[:, 0, :, :], M1v[:, :, 0, :], M1v[:, :, 1, :], OP.subtract, r=[b_M], w=[b_qk])
            k.tt("pool", qk[:, 1, :, :], M2v[:, :, 0, :], M2v[:, :, 1, :], OP.add, r=[b_M], w=[b_qk])
            if cut == 4:
                break
            k.act(v_r, PS[5][:, :], AF.Copy, r=[PB[5]], w=[b_vr])
            k.act(sig, PS[6][:, :], AF.Sigmoid, r=[PB[6]], w=[b_sig])
            k.tt("dve", sg, PS[6][:, :], sig, OP.mult, r=[PB[6], b_sig], w=[b_sg])
            if cut == 5:
                break
            for h in range(2):
                k.mm(PS[0][:, 0:128], qk[:, h, 1, :], qk[:, h, 0, :], h == 0, h == 1,
                     r=[b_qk], w=[PB[0]])
            for h in range(2):
                k.mm(PS[1][:, h * 128:(h + 1) * 128], qk[:, h, 1, :], ident, True, True,
                     r=[b_qk, b_const], w=[PB[1]])
            k.tt("dve", At, PS[0][:, 0:128], dmaskT, OP.mult, r=[PB[0], b_const], w=[b_At])
            k.act(kdec, PS[1][:, 0:256], AF.Copy, r=[PB[1], b_const], w=[b_kdec], scale=cols[:, 0:1])
            k.tt("pool", qdT[:, 0, :], qk[:, 0, 0, :], qdec, OP.mult, r=[b_qk, b_const], w=[b_qdT])
            k.tt("pool", qdT[:, 1, :], qk[:, 1, 0, :], qdec, OP.mult, r=[b_qk, b_const], w=[b_qdT])
            k.mm(PS[0][:, :], At, v_r, True, False, r=[b_At, b_vr], w=[PB[0]])
            for h in range(2):
                k.mm(PS[0][:, :], qdT[:, h, :], Sbf[h], False, h == 1, r=[b_qdT, b_Sbf[h]], w=[PB[0]])
            if cut == 6:
                break
            for h in range(2):
                pb = 1 if h == 0 else 5
                k.mm(PS[pb][:, :], kdec[:, h * 128:(h + 1) * 128], v_r, True, True,
                     r=[b_kdec, b_vr], w=[PB[pb]])
                k.stt(Sr[h], Sr[h], cols[:, 1:2], PS[pb][:, :], OP.mult, OP.add,
                      r=[b_Sr[h], PB[pb], b_const], w=[b_Sr[h]])
                k.cp("pool", Sbf[h], Sr[h], r=[b_Sr[h]], w=[b_Sbf[h]])
            ssr, sqr, rr = stat[:, 3:4], stat[:, 4:5], stat[:, 5:6]
            k.act(sqj[:, 0:512], PS[0][:, :], AF.Square, r=[PB[0]], w=[b_sq, b_stat], accum_out=ssr)
            k.act(sqr, ssr, AF.Sqrt, r=[b_stat], w=[b_stat], bias=EPS, scale=1.0 / 512)
            k.recip(rr, sqr, r=[b_stat], w=[b_stat])
            k.stt(ost[s][:, 256:768], PS[0][:, :], rr, sg, OP.mult, OP.mult,
                  r=[PB[0], b_stat, b_sg], w=[b_ost[s]])

            if cut == 7:
                break
            k.act(sig_tm, PS[4][:, 0:256], AF.Sigmoid, r=[PB[4]], w=[b_tm])
            k.tt("pool", sig_tm, sig_tm, oml_bc, OP.mult, r=[b_tm, b_lb], w=[b_tm])
            k.tt("pool", sig_tm, sig_tm, lb_bc, OP.add, r=[b_tm, b_lb], w=[b_tm])
            k.act(g_tm, sig_tm, AF.Ln, r=[b_tm], w=[b_g])
            k.cp("dve", vh, PS[4][:, 256:512], r=[PB[4]], w=[b_vh])
            k.act(sgn, PS[2][:, 256:512], AF.Sigmoid, r=[PB[2]], w=[b_sgn], scale=-1.0)
            k.act(sigh, PS[7][:, 0:256], AF.Sigmoid, r=[PB[7]], w=[b_sigh])
            k.tt("dve", ghw, PS[7][:, 0:256], sigh, OP.mult, r=[PB[7], b_sigh], w=[b_ghw])
            k.tt("pool", ghw, ghw, nw_bc, OP.mult, r=[b_ghw, b_lb], w=[b_ghw])
            if cut == 8:
                break
            k.cp("dve", g_hi, g_tm, r=[b_g], w=[b_ghl])
            k.tt("dve", g_lo, g_tm, g_hi, OP.subtract, r=[b_g, b_ghl], w=[b_ghl])
            for j in range(2):
                k.mm(PS[6][:, j * 256:j * 256 + 130], g_hi[:, j * 128:(j + 1) * 128], trimid, True, False,
                     r=[b_ghl, b_const], w=[PB[6]])
                k.mm(PS[6][:, j * 256:j * 256 + 130], g_lo[:, j * 128:(j + 1) * 128], trimid, False, True,
                     r=[b_ghl, b_const], w=[PB[6]])
            for j in range(2):
                k.act(EB[:, j, :], PS[6][:, j * 256:j * 256 + 128], AF.Exp, r=[PB[6]], w=[b_EB])
                k.act(ENB[:, j, :], PS[6][:, j * 256:j * 256 + 130], AF.Exp, r=[PB[6]], w=[b_ENB], scale=-1.0)
            for j in range(2):
                k.stt(qdTh[:, j, :], PS[2][:, j * 128:(j + 1) * 128], float(128 ** -0.5), EB[:, j, :],
                      OP.mult, OP.mult, r=[PB[2], b_EB], w=[b_qdTh])
                k.stt(kdTh[:, j, :], sgn[:, j * 128:(j + 1) * 128], omlcol[:, j:j + 1], ENB[:, j, 0:128],
                      OP.mult, OP.mult, r=[b_sgn, b_ENB, b_lb], w=[b_kdTh])
            if cut == 9:
                break
            for j in range(2):
                k.mm(PS[7][:, 256 + j * 128:256 + (j + 1) * 128], kdTh[:, j, :], qdTh[:, j, :], True, True,
                     r=[b_kdTh, b_qdTh], w=[PB[7]])
            for j in range(2):
                k.tt("dve", Ath[:, j, :], PS[7][:, 256 + j * 128:256 + (j + 1) * 128], causT, OP.mult,
                     r=[PB[7], b_const], w=[b_Ath])
            for j in range(2):
                k.mm(PS[3][:, j * 128:(j + 1) * 128], kdTh[:, j, :], ident, True, True,
                     r=[b_kdTh, b_const], w=[PB[3]])
            k.cp("dve", kdt, PS[3][:, 0:256], r=[PB[3]], w=[b_kdt])
            for j in range(2):
                k.ts("dve", Spb[:, j, :], Sh[:, j, :], ENB[:, j, 128:129], None, OP.mult,
                     r=[b_Sh[j], b_ENB], w=[b_Spb[j]])
            if cut == 10:
                break
            for j in range(2):
                k.mm(PS[4][:, j * 128:(j + 1) * 128], Ath[:, j, :], vh[:, j * 128:(j + 1) * 128], True, False,
                     r=[b_Ath, b_vh], w=[PB[4]])
                k.mm(PS[4][:, j * 128:(j + 1) * 128], qdTh[:, j, :], Spb[:, j, :], False, True,
                     r=[b_qdTh, b_Spb[j]], w=[PB[4]])
            for j in range(2):
                k.mm(PS[3][:, 256 + j * 128:256 + (j + 1) * 128], kdt[:, j * 128:(j + 1) * 128],
                     vh[:, j * 128:(j + 1) * 128], True, True, r=[b_kdt, b_vh], w=[PB[3]])
            for j in range(2):
                k.stt(tmpS[:, j, :], Sh[:, j, :], ENB[:, j, 128:129], PS[3][:, 256 + j * 128:256 + (j + 1) * 128],
                      OP.mult, OP.add, r=[b_Sh[j], b_ENB, PB[3]], w=[b_tmpS[j]])
                k.ts("dve", Sh[:, j, :], tmpS[:, j, :], EB[:, j, 127:128], None, OP.mult,
                     r=[b_tmpS[j], b_EB], w=[b_Sh[j]])
            for j in range(2):
                ssh, sqh, rh = stat[:, 6 + 3 * j:7 + 3 * j], stat[:, 7 + 3 * j:8 + 3 * j], stat[:, 8 + 3 * j:9 + 3 * j]
                k.act(sqj[:, 512 + j * 128:512 + (j + 1) * 128], PS[4][:, j * 128:(j + 1) * 128], AF.Square,
                      r=[PB[4]], w=[b_sq, b_stat], accum_out=ssh)
                k.act(sqh, ssh, AF.Sqrt, r=[b_stat], w=[b_stat], bias=EPS, scale=1.0 / 128)
                k.recip(rh, sqh, r=[b_stat], w=[b_stat])
                k.stt(ost[s][:, j * 128:(j + 1) * 128], PS[4][:, j * 128:(j + 1) * 128], rh,
                      ghw[:, j * 128:(j + 1) * 128], OP.mult, OP.mult,
                      r=[PB[4], b_stat, b_ghw], w=[b_ost[s]])
            if stage == 1:
                k.ld("sp", y1[i * 128:(i + 1) * 128, :], ost[s], r=[b_ost[s]], w=[b_obuf])
            else:
                k.ld("sp", obuf[i * 128:(i + 1) * 128, :], ost[s], r=[b_ost[s]], w=[b_obuf])


        if stage == 2 and not cut:
            phase2(nc, es, S, k, a16, a32, PS, PB, ident, b_const, obuf, b_obuf, gath, x1buf, xo, oidx_d,
                   wg_d, wbh_d, wbr_d, wo_d, nmw_t, nfw_d, fnw_d, wq_d, skT_d, eu_d, ev_d, iota_d, yout, nt2, mock)
        if cut:
            k.ld("sp", y1[0:128, :], ost[0], r=[b_ost[0]], w=[b_obuf])
        S.finish()
        with nc.Block() as block:
            S.emit(block)
    return nc


def _bf(a):
    return np.ascontiguousarray(a).astype(ml_dtypes.bfloat16)


def make_inputs2(core, x, w_in, w_branch_hg, w_branch_ret, w_out, norm_ffn_w, peer_w_q, peer_sub_keys,
                 expert_u, expert_v, final_norm_w, **kw):
    b, g = divmod(core, 4)
    p = np.arange(128, dtype=np.int64)[:, None]
    col = np.arange(64, dtype=np.int64)[None, :]
    i_, r_ = col // 4, col % 4
    oidx = ((b * 4 + r_) * SEQ + g * 2048 + i_ * 128 + p).astype(np.uint32)
    f = lambda a: np.ascontiguousarray(a, dtype=np.float32)
    skT = f(np.transpose(peer_sub_keys[0].reshape(16, 128, 128), (2, 0, 1)).reshape(128, 2048))
    return {
        "xo": f(x[b, g * 2048:(g + 1) * 2048]),
        "oidx": np.ascontiguousarray(oidx),
        "wg": f(w_in[0][:, 10240:12288]),
        "wbh": f(w_branch_hg[0]), "wbr": f(w_branch_ret[0]), "wo": f(w_out[0]),
        "nfw": f(norm_ffn_w[0].reshape(1, D)), "fnw": f(final_norm_w.reshape(1, D)),
        "wq": f(peer_w_q[0]), "skT": skT,
        "eu": f(expert_u[0]), "ev": f(expert_v[0]),
        "iota": f(np.broadcast_to(np.arange(128, dtype=np.float32)[None, :], (128, 128))),
    }


def make_inputs(core, x, norm_mix_w, w_in, hg_lower_bounds, hg_norm_w, **kw):
    b, g = divmod(core, 4)
    W = w_in[0]
    h0, h1 = 2 * g, 2 * g + 1

    def hcol(base, h, n=128):
        return W[:, base + h * n: base + (h + 1) * n]
    HQ, HF, HI, HG = 0, 1024, 2048, 3072
    RQ, RK, RV, RG = 4096, 5120, 6144, 8192
    fm = [hcol(HQ, h0), hcol(HQ, h1), hcol(HF, h0), hcol(HF, h1),
          W[:, RQ + g * 256: RQ + (g + 1) * 256], W[:, RK + g * 256: RK + (g + 1) * 256]]
    tm = [hcol(HF, h0), hcol(HF, h1), hcol(HI, h0), hcol(HI, h1),
          W[:, RV + g * 512: RV + (g + 1) * 512], W[:, RG + g * 512: RG + (g + 1) * 512],
          hcol(HG, h0), hcol(HG, h1)]
    w1 = np.ascontiguousarray(np.concatenate(fm + tm, axis=1), dtype=np.float32)
    assert w1.shape == (1024, 2816)
    lb = hg_lower_bounds
    lbr = np.ascontiguousarray(lb[:, h0 * 128:(h1 + 1) * 128], dtype=np.float32)
    lbc = np.ascontiguousarray(np.stack([lb[0, h0 * 128:(h0 + 1) * 128], lb[0, h1 * 128:(h1 + 1) * 128],
                                         lb[1, h0 * 128:(h0 + 1) * 128], lb[1, h1 * 128:(h1 + 1) * 128]], axis=1),
                               dtype=np.float32)
    inv = (10000.0 ** (-np.arange(128, dtype=np.float32) / np.float32(128))).astype(np.float32)
    pos = np.arange(SEQ, dtype=np.float32)
    ang = (pos[None, :] * inv[:, None]).astype(np.float32)
    cosT = np.cos(ang).astype(np.float32).reshape(128, NT1_FULL, 128)
    sinT = np.sin(ang).astype(np.float32).reshape(128, NT1_FULL, 128)
    rot = np.ascontiguousarray(np.stack([cosT, sinT, cosT], axis=2).reshape(128, NT1_FULL, 384))
    log_g = np.log(np.float32(1.0) - np.exp2(np.float32(-5.0 - g))).astype(np.float32)
    idx = np.arange(128, dtype=np.float32)
    rel = idx[None, :] - idx[:, None]
    dmaskT = np.where(rel >= 0, np.exp(log_g * np.maximum(rel, 0.0)), 0.0).astype(np.float32) / np.float32(16.0)
    qdec = np.broadcast_to(np.exp(log_g * (idx + 1.0)).astype(np.float32)[None, :], (128, 128)).copy()
    cols = np.zeros((128, 4), np.float32)
    cols[:, 0] = np.exp(log_g * (127.0 - idx)) / 16.0
    cols[:, 1] = np.exp(log_g * 128.0)
    tri = np.zeros((128, 130), np.float32)
    sidx = np.arange(128)
    for t in range(128):
        tri[:, t] = (sidx <= t).astype(np.float32) - (sidx <= 63).astype(np.float32)
    tri[:, 128] = -(sidx <= 63).astype(np.float32)
    causT = (sidx[:, None] <= sidx[None, :]).astype(np.float32)
    return {
        "xb": np.ascontiguousarray(x[b], dtype=np.float32),
        "w1": w1,
        "nmw": np.ascontiguousarray(norm_mix_w[0].reshape(8, 128).T, dtype=np.float32),
        "lbr": lbr, "lbc": lbc,
        "hnw": np.ascontiguousarray(hg_norm_w[0, h0 * 128:(h1 + 1) * 128].reshape(1, 256), dtype=np.float32),
        "ident": _bf(np.eye(128, dtype=np.float32)),
        "rot": rot, "dmaskT": dmaskT, "qdec": qdec, "cols": cols, "trimid": _bf(tri), "causT": causT,
    }


def kernel(**inputs):
    inputs = {k_: np.asarray(v) for k_, v in inputs.items()}
    nc = build_nc()
    in_maps = []
    for c in range(NCORE):
        m = make_inputs(c, **inputs)
        m.update(make_inputs2(c, **inputs))
        in_maps.append(m)
    res = run_bass_kernel_spmd(nc, in_maps, core_ids=list(range(NCORE)))
    out = np.zeros((2, SEQ, D), np.float32)
    for c in range(NCORE):
        b, g = divmod(c, 4)
        out[b, g * 2048:(g + 1) * 2048] = res.results[c]["y"]
    return out
```

```python
import numpy as np
import ml_dtypes
from contextlib import ExitStack
import concourse.bass as bass
import concourse.mybir as mybir
from concourse.bass_utils import run_bass_kernel_spmd

F32 = mybir.dt.float32
BF = mybir.dt.bfloat16
U32 = mybir.dt.uint32
AF = mybir.ActivationFunctionType
OP = mybir.AluOpType
AX = mybir.AxisListType

D = 1024
SEQ = 8192
NCORE = 8
EPS = 1e-6
NT1_FULL = SEQ // 128
NT2_FULL = 16


class Buf:
    __slots__ = ("w", "r", "excl")

    def __init__(self, excl=False):
        self.w = None
        self.r = {}
        self.excl = excl


class Sched:
    ENG = ("pe", "act", "dve", "pool", "sp")

    def __init__(self, nc, es, ndma=48):
        self.nc = nc
        self.sem = {e: es.enter_context(nc.semaphore("s_" + e)) for e in self.ENG}
        self.cnt = {e: 0 for e in self.ENG}
        self.prog = {e: [] for e in self.ENG}
        self.waited = {e: {} for e in self.ENG}
        self.dsem = [es.enter_context(nc.semaphore("d%d" % i)) for i in range(ndma + 1)]
        self.dcnt = [0] * (ndma + 1)
        self.dnext = 0
        self.ndma = ndma
        self.npool0 = 32
        self.pnext = 0

    def _semof(self, key):
        return self.sem[key] if isinstance(key, str) else self.dsem[key]

    def _wait(self, e, key, val):
        if self.waited[e].get(key, 0) >= val:
            return
        self.waited[e][key] = val
        self.prog[e].append(("w", self._semof(key), val))

    def _need(self, e, tok):
        k, v = tok
        if k == e and e in ("pe", "sp"):
            return
        self._wait(e, k, v)

    def _deps(self, e, reads, writes):
        for b in reads:
            if b.w is not None:
                self._need(e, b.w)
        for b in writes:
            if b.w is not None:
                self._need(e, b.w)
            for k, v in b.r.items():
                self._need(e, (k, v))

    def _mark(self, tok, reads, writes):
        k, v = tok
        for b in reads:
            if b.r.get(k, 0) < v:
                b.r[k] = v
        for b in writes:
            b.w = tok
            b.r = {}

    def op(self, e, fn, reads=(), writes=()):
        if any(b.excl for b in reads):
            writes = list(writes) + [b for b in reads if b.excl and b not in writes]
            reads = [b for b in reads if not b.excl]
        self._deps(e, reads, writes)
        self.cnt[e] += 1
        self.prog[e].append(("i", fn, self.sem[e], 1))
        self._mark((e, self.cnt[e]), reads, writes)

    def dma(self, q, fn, reads=(), writes=(), inc=16):
        self._deps(q, reads, writes)
        if q == "pool":
            i = self.npool0 + self.pnext
            self.pnext = (self.pnext + 1) % (self.ndma - self.npool0)
        else:
            i = self.dnext
            self.dnext = (self.dnext + 1) % self.npool0
        if self.dcnt[i] > 0:
            self._wait(q, i, self.dcnt[i])
        self.dcnt[i] += inc
        self.prog[q].append(("i", fn, self.dsem[i], inc))
        self._mark((i, self.dcnt[i]), reads, writes)

    def coll(self, fn, reads=(), writes=()):
        self._deps("pool", reads, writes)
        i = len(self.dsem) - 1
        self.dcnt[i] += 1
        self.prog["pool"].append(("i", fn, self.dsem[i], 1))
        self._mark((i, self.dcnt[i]), reads, writes)

    def barrier(self):
        for e in self.ENG:
            for k in self.ENG:
                if k != e and self.cnt[k] > 0:
                    self._wait(e, k, self.cnt[k])
            for i, c in enumerate(self.dcnt):
                if c > 0:
                    self._wait(e, i, c)

    def finish(self):
        for i, c in enumerate(self.dcnt):
            if c > 0:
                self._wait("sp", i, c)
        for k in self.ENG:
            if k != "sp" and self.cnt[k] > 0:
                self._wait("sp", k, self.cnt[k])

    def emit(self, block):
        def mk(e):
            def body(eng):
                for it in self.prog[e]:
                    if it[0] == "w":
                        eng.wait_ge(it[1], it[2])
                    else:
                        it[1](eng).then_inc(it[2], it[3])
            return body
        block.tensor(mk("pe"))
        block.scalar(mk("act"))
        block.vector(mk("dve"))
        block.gpsimd(mk("pool"))
        block.sync(mk("sp"))


class Arena:
    def __init__(self, t, n):
        self.t = t
        self.n = n
        self.off = 0
        self.mark = 0

    def get(self, n):
        assert self.off + n <= self.n, ("arena overflow", self.off, n, self.n)
        ap = self.t[:, self.off:self.off + n]
        self.off += n
        return ap


class K:
    def __init__(self, S):
        self.S = S

    def mm(self, out, lhsT, rhs, start, stop, r=(), w=()):
        self.S.op("pe", lambda e: e.matmul(out, lhsT, rhs, start=start, stop=stop), r, w)

    def act(self, out, in_, func, r=(), w=(), bias=0.0, scale=1.0, accum_out=None):
        if accum_out is None:
            self.S.op("act", lambda e: e.activation(out, in_, func, bias=bias, scale=scale), r, w)
        else:
            self.S.op("act", lambda e: e.activation(out, in_, func, bias=bias, scale=scale,
                                                    accum_out=accum_out), r, w)

    def ts(self, eng, out, in0, s1, s2, op0, op1=None, r=(), w=()):
        if op1 is None:
            self.S.op(eng, lambda e: e.tensor_scalar(out, in0, s1, None, op0), r, w)
        else:
            self.S.op(eng, lambda e: e.tensor_scalar(out, in0, s1, s2, op0, op1), r, w)

    def tt(self, eng, out, in0, in1, op, r=(), w=()):
        self.S.op(eng, lambda e: e.tensor_tensor(out, in0, in1, op), r, w)

    def stt(self, out, in0, scalar, in1, op0, op1, r=(), w=(), accum_out=None):
        if accum_out is None:
            self.S.op("dve", lambda e: e.scalar_tensor_tensor(out, in0, scalar, in1, op0, op1), r, w)
        else:
            self.S.op("dve", lambda e: e.scalar_tensor_tensor(out, in0, scalar, in1, op0, op1,
                                                              accum_out=accum_out), r, w)

    def cp(self, eng, out, in_, r=(), w=()):
        self.S.op(eng, lambda e: e.tensor_copy(out, in_), r, w)

    def recip(self, out, in_, r=(), w=()):
        self.S.op("dve", lambda e: e.reciprocal(out, in_), r, w)

    def ld(self, q, out, in_, r=(), w=(), slow=False):
        if slow:
            self.S.dma(q, lambda e: e.dma_start(out=out, in_=in_, allow_slow_non_contiguous=True), r, w)
        else:
            self.S.dma(q, lambda e: e.dma_start(out=out, in_=in_), r, w)


def load_w(k, stg, b_stg, dst, b_dst, src, nk, ncol, ci0=0):
    srcv = src.rearrange("(k p) n -> p k n", p=128)
    ci = ci0
    step = 1024
    for kk in range(nk):
        for c0 in range(0, ncol, step):
            s = ci % 2
            k.ld("sp", stg[s][:, 0:step], srcv[:, kk, c0:c0 + step], w=[b_stg[s]])
            eng = ("dve", "pool", "act")[ci % 3]
            d = dst[:, kk, c0:c0 + step]
            if eng == "act":
                k.act(d, stg[s][:, 0:step], AF.Copy, r=[b_stg[s]], w=[b_dst])
            else:
                k.cp(eng, d, stg[s][:, 0:step], r=[b_stg[s]], w=[b_dst])
            ci += 1
    return ci


def phase2(nc, es, S, k, a16, a32, PS, PB, ident, b_const, obuf, b_obuf, gath, x1buf, xo, oidx_d,
           wg_d, wbh_d, wbr_d, wo_d, nmw_t, nfw_d, fnw_d, wq_d, skT_d, eu_d, ev_d, iota_d, yout, nt2, mock):
    S.barrier()
    b_gath = Buf()
    if not mock:
        S.coll(lambda e: e.collective_compute("AllGather", OP.bypass, replica_groups=[list(range(NCORE))],
                                              ins=[obuf[:, :]], outs=[gath[:, :]]), [b_obuf], [b_gath])
    a16.off, a32.off = a16.mark, a32.mark
    oidx = es.enter_context(nc.sbuf_tensor("oidx_sb", [128, 64], U32))
    tiu = es.enter_context(nc.sbuf_tensor("tiu", [128, 256], U32))
    eidx = es.enter_context(nc.sbuf_tensor("eidx", [128, 128], U32))
    b_oidx = Buf()
    k.ld("sp", oidx[:, :], oidx_d[:, :], w=[b_oidx])

    m16, m32 = a16.off, a32.off
    wg = a16.get(8 * 2048).rearrange("p (k n) -> p k n", k=8)
    wbh = a16.get(8 * 1024).rearrange("p (k n) -> p k n", k=8)
    wbr = a16.get(16 * 1024).rearrange("p (k n) -> p k n", k=16)
    wo = a16.get(8 * 1024).rearrange("p (k n) -> p k n", k=8)
    b_w2 = Buf()
    stg = [a32.get(1024) for _ in range(2)]
    b_stg = [Buf(), Buf()]
    ci = load_w(k, stg, b_stg, wg, b_w2, wg_d, 8, 2048)
    ci = load_w(k, stg, b_stg, wbh, b_w2, wbh_d, 8, 1024, ci)
    ci = load_w(k, stg, b_stg, wbr, b_w2, wbr_d, 16, 1024, ci)
    ci = load_w(k, stg, b_stg, wo, b_w2, wo_d, 8, 1024, ci)
    xt = [a32.get(1024) for _ in range(2)]
    b_xt = [Buf(), Buf()]
    sqj = a16.get(1024)
    xn = a16.get(1024)
    hT = a16.get(1024).rearrange("p (k t) -> p k t", k=8)
    b_sq, b_xn, b_hT = Buf(), Buf(), Buf()
    stat = a32.get(16)
    b_stat = Buf()
    sgate = a32.get(2048)
    b_sgate = Buf()
    ot = a16.get(3072).rearrange("p (r f) -> p r f", r=4)
    b_ot = Buf()
    oT = a16.get(3072).rearrange("p (c t) -> p c t", c=24)
    b_oT = Buf()
    m1 = a32.get(1024)
    b_m1 = Buf()
    mbf = a16.get(1024)
    b_mbf = Buf()
    mT = a16.get(1024).rearrange("p (k t) -> p k t", k=8)
    b_mT = Buf()
    x1 = a32.get(1024)
    b_x1 = Buf()
    b_x1buf = Buf()

    def norm_T(src, b_src, use_nmw):
        ss, sq_, rstd = stat[:, 0:1], stat[:, 1:2], stat[:, 2:3]
        k.act(sqj, src, AF.Square, r=[b_src], w=[b_sq, b_stat], accum_out=ss)
        k.act(sq_, ss, AF.Sqrt, r=[b_stat], w=[b_stat], bias=EPS, scale=1.0 / D)
        k.recip(rstd, sq_, r=[b_stat], w=[b_stat])
        k.ts("dve", xn, src, rstd, None, OP.mult, r=[b_src, b_stat], w=[b_xn])
        for c in range(8):
            pb = c // 4
            k.mm(PS[pb][:, (c % 4) * 128:(c % 4 + 1) * 128], xn[:, c * 128:(c + 1) * 128], ident,
                 True, True, r=[b_xn, b_const], w=[PB[pb]])
        for c in range(8):
            pb = c // 4
            srcp = PS[pb][:, (c % 4) * 128:(c % 4 + 1) * 128]
            k.ts("dve", hT[:, c, :], srcp, use_nmw[:, c:c + 1], None, OP.mult, r=[PB[pb], b_const], w=[b_hT])

    for i in range(nt2):
        s = i % 2
        k.ld("sp", xt[s], xo[i * 128:(i + 1) * 128, :], w=[b_xt[s]])
        for r in range(4):
            src_rows = gath[:, :]
            if mock:
                k.ld("sp", ot[:, r, :], obuf[i * 128:(i + 1) * 128, :], r=[b_gath, b_obuf], w=[b_ot])
            else:
                S.dma("pool", (lambda o_, c_: (lambda e: e.indirect_dma_start(
                    o_, None, src_rows, bass.IndirectOffsetOnAxis(ap=oidx[:, c_:c_ + 1], axis=0))))(ot[:, r, :], i * 4 + r),
                    [b_gath, b_oidx], [b_ot])
        norm_T(xt[s], b_xt[s], nmw_t)
        for j in range(4):
            pb = 2 + j
            for kk in range(8):
                k.mm(PS[pb][:, :], hT[:, kk, :], wg[:, kk, j * 512:(j + 1) * 512], kk == 0, kk == 7,
                     r=[b_hT, b_w2], w=[PB[pb]])
            k.act(sgate[:, j * 512:(j + 1) * 512], PS[pb][:, :], AF.Sigmoid, r=[PB[pb]], w=[b_sgate])
        for c in range(24):
            pb = 6 + (c // 4) % 2
            r_, f_ = divmod(c, 6)
            k.mm(PS[pb][:, (c % 4) * 128:(c % 4 + 1) * 128], ot[:, r_, f_ * 128:(f_ + 1) * 128], ident,
                 True, True, r=[b_ot, b_const], w=[PB[pb]])
            if c % 4 == 3:
                c0 = c - 3
                k.cp("dve", oT[:, c0:c0 + 4, :], PS[pb][:, :].rearrange("p (c t) -> p c t", c=4),
                     r=[PB[pb]], w=[b_oT])
        for half in range(2):
            n = 0
            for r_ in range(4):
                for j in range(2):
                    k.mm(PS[half][:, :], oT[:, r_ * 6 + j, :], wbh[:, 2 * r_ + j, half * 512:(half + 1) * 512],
                         n == 0, n == 7, r=[b_oT, b_w2], w=[PB[half]])
                    n += 1
            n = 0
            for r_ in range(4):
                for m in range(4):
                    k.mm(PS[2 + half][:, :], oT[:, r_ * 6 + 2 + m, :], wbr[:, r_ * 4 + m, half * 512:(half + 1) * 512],
                         n == 0, n == 15, r=[b_oT, b_w2], w=[PB[2 + half]])
                    n += 1
        for half in range(2):
            cs = slice(half * 512, (half + 1) * 512)
            k.tt("dve", m1[:, cs], PS[half][:, :], sgate[:, half * 512:(half + 1) * 512], OP.mult,
                 r=[PB[half], b_sgate], w=[b_m1])
            k.tt("dve", mbf[:, cs], PS[2 + half][:, :], sgate[:, 1024 + half * 512:1024 + (half + 1) * 512], OP.mult,
                 r=[PB[2 + half], b_sgate], w=[b_mbf])
            k.tt("dve", mbf[:, cs], mbf[:, cs], m1[:, cs], OP.add, r=[b_mbf, b_m1], w=[b_mbf])
        for c in range(8):
            pb = 4 + c // 4
            k.mm(PS[pb][:, (c % 4) * 128:(c % 4 + 1) * 128], mbf[:, c * 128:(c + 1) * 128], ident, True, True,
                 r=[b_mbf, b_const], w=[PB[pb]])
            if c % 4 == 3:
                c0 = c - 3
                k.cp("dve", mT[:, c0:c0 + 4, :], PS[pb][:, :].rearrange("p (c t) -> p c t", c=4),
                     r=[PB[pb]], w=[b_mT])
        for half in range(2):
            pb = 6 + half
            for kk in range(8):
                k.mm(PS[pb][:, :], mT[:, kk, :], wo[:, kk, half * 512:(half + 1) * 512], kk == 0, kk == 7,
                     r=[b_mT, b_w2], w=[PB[pb]])
            k.tt("dve", x1[:, half * 512:(half + 1) * 512], PS[pb][:, :], xt[s][:, half * 512:(half + 1) * 512], OP.add,
                 r=[PB[pb], b_xt[s]], w=[b_x1])
        k.ld("sp", x1buf[i * 128:(i + 1) * 128, :], x1, r=[b_x1], w=[b_x1buf])

    S.barrier()
    a16.off, a32.off = m16, m32
    wq = a16.get(8 * 2048).rearrange("p (k n) -> p k n", k=8)
    skT = a16.get(2048).rearrange("p (g n) -> p g n", g=16)
    b_w3 = Buf()
    stg = [a32.get(1024) for _ in range(2)]
    b_stg = [Buf(), Buf()]
    load_w(k, stg, b_stg, wq, b_w3, wq_d, 8, 2048)
    for hh in range(2):
        k.ld("sp", stg[hh], skT_d[:, hh * 1024:(hh + 1) * 1024], w=[b_stg[hh]])
        k.cp("dve", skT[:, hh * 8:(hh + 1) * 8, :], stg[hh].rearrange("p (g n) -> p g n", g=8), r=[b_stg[hh]], w=[b_w3])
    nfw_bc = a32.get(1024)
    fnw_bc = a32.get(1024)
    iota = a32.get(128)
    b_c3 = Buf()
    k.ld("sp", nfw_bc, nfw_d[0:1, :].partition_broadcast(128), w=[b_c3])
    k.ld("sp", fnw_bc, fnw_d[0:1, :].partition_broadcast(128), w=[b_c3])
    k.ld("sp", iota, iota_d[:, :], w=[b_c3])
    x1t = a32.get(1024)
    b_x1t = Buf()
    h2 = a32.get(1024)
    b_h2 = Buf()
    hbf = a16.get(1024)
    b_hbf = Buf()
    hT = a16.get(1024).rearrange("p (k t) -> p k t", k=8)
    b_hT = Buf()
    qT = a16.get(2048).rearrange("p (g t) -> p g t", g=16)
    b_qT = Buf()
    junk = a16.get(1024)
    b_junk = Buf()
    stat = a32.get(32)
    b_stat = Buf()
    sc = a32.get(2048).rearrange("p (g n) -> p g n", g=16)
    b_sc = Buf()
    scr = a32.get(256)
    b_scr = Buf()
    tv = a32.get(256).rearrange("p (g n) -> p g n", g=16)
    tif = a32.get(256).rearrange("p (g n) -> p g n", g=16)
    b_tv, b_ti, b_tif = Buf(), Buf(), Buf()
    cand = a32.get(256)
    cidx = a32.get(256)
    b_cand, b_cidx = Buf(), Buf()
    bv = a32.get(128).rearrange("p (h n) -> p h n", h=8)
    ex = a32.get(128).rearrange("p (h n) -> p h n", h=8)
    b_bv, b_ex = Buf(), Buf()
    ef = a32.get(128)
    b_ef = Buf()
    b_eidx = Buf()
    dots = a32.get(128)
    b_dots = Buf()
    g1 = a32.get(128)
    g2 = a32.get(128)
    wgt = a32.get(128)
    b_g, b_wgt = Buf(), Buf()
    NB = 8
    gbuf = es.enter_context(nc.sbuf_tensor("gbuf", [128, 4096], F32))
    ub = [a32.get(1024) for _ in range(2)] + [gbuf[:, 0:1024], gbuf[:, 1024:2048]]
    vb = [a32.get(1024) for _ in range(2)] + [gbuf[:, 2048:3072], gbuf[:, 3072:4096]]
    ub += [a16.get(2048).bitcast(F32) for _ in range(4)]
    vb += [a16.get(2048).bitcast(F32) for _ in range(4)]
    assert tuple(ub[-1].shape) == (128, 1024), ub[-1].shape
    b_ub = [Buf() for _ in range(NB)]
    b_vb = [Buf() for _ in range(NB)]
    acc = a32.get(1024)
    b_acc = Buf()
    b_y = Buf()
    vbf = [a16.get(1024) for _ in range(2)]
    b_vbf = [Buf(), Buf()]
    dg = [a16.get(128) for _ in range(2)]
    b_dg = [Buf(), Buf()]
    tvu = tiu[:, :].rearrange("p (g n) -> p g n", g=16)
    fz = a32.get(8)
    b_fz = Buf()
    S.op("pool", (lambda ap: (lambda e: e.memset(ap, 0.0)))(fz), (), [b_fz])
    NEG = -3.0e38

    for i in range(nt2):
        k.ld("sp", x1t, x1buf[i * 128:(i + 1) * 128, :], r=[b_x1buf], w=[b_x1t])
        ss, sq_, rstd = stat[:, 0:1], stat[:, 1:2], stat[:, 2:3]
        k.act(junk, x1t, AF.Square, r=[b_x1t], w=[b_junk, b_stat], accum_out=ss)
        k.act(sq_, ss, AF.Sqrt, r=[b_stat], w=[b_stat], bias=EPS, scale=1.0 / D)
        k.recip(rstd, sq_, r=[b_stat], w=[b_stat])
        k.stt(h2, x1t, rstd, nfw_bc, OP.mult, OP.mult, r=[b_x1t, b_stat, b_c3], w=[b_h2])
        k.cp("dve", hbf, h2, r=[b_h2], w=[b_hbf])
        for c in range(8):
            pb = c // 4
            k.mm(PS[pb][:, (c % 4) * 128:(c % 4 + 1) * 128], hbf[:, c * 128:(c + 1) * 128], ident,
                 True, True, r=[b_hbf, b_const], w=[PB[pb]])
            if c % 4 == 3:
                c0 = c - 3
                k.cp("dve", hT[:, c0:c0 + 4, :], PS[pb][:, :].rearrange("p (c t) -> p c t", c=4),
                     r=[PB[pb]], w=[b_hT])
        for gq in range(16):
            pb = 2 + gq // 4
            for kk in range(8):
                k.mm(PS[pb][:, (gq % 4) * 128:(gq % 4 + 1) * 128], wq[:, kk, gq * 128:(gq + 1) * 128], hT[:, kk, :],
                     kk == 0, kk == 7, r=[b_w3, b_hT], w=[PB[pb]])
            if gq % 4 == 3:
                g0 = gq - 3
                k.cp("dve", qT[:, g0:g0 + 4, :], PS[pb][:, :].rearrange("p (c t) -> p c t", c=4),
                     r=[PB[pb]], w=[b_qT])
        for gq in range(16):
            pb = 6 + (gq // 4) % 2
            k.mm(PS[pb][:, (gq % 4) * 128:(gq % 4 + 1) * 128], qT[:, gq, :], skT[:, gq, :], True, True,
                 r=[b_qT, b_w3], w=[PB[pb]])
            if gq % 4 == 3:
                g0 = gq - 3
                k.cp("dve", sc[:, g0:g0 + 4, :], PS[pb][:, :].rearrange("p (c t) -> p c t", c=4),
                     r=[PB[pb]], w=[b_sc])
        for gq in range(16):
            S.op("dve", (lambda o_, i_: (lambda e: e.max(o_, i_)))(tv[:, gq, 0:8], sc[:, gq, :]), [b_sc], [b_tv])
            S.op("dve", (lambda o_, m_, i_: (lambda e: e.max_index(o_, m_, i_)))(tvu[:, gq, 0:8], tv[:, gq, 0:8], sc[:, gq, :]),
                 [b_sc, b_tv], [b_ti])
            S.op("dve", (lambda o_, m_, i_: (lambda e: e.match_replace(o_, m_, i_, NEG)))(scr[:, 0:128], tv[:, gq, 0:8], sc[:, gq, :]),
                 [b_sc, b_tv], [b_scr])
            S.op("dve", (lambda o_, i_: (lambda e: e.max(o_, i_)))(tv[:, gq, 8:16], scr[:, 0:128]), [b_scr], [b_tv])
            S.op("dve", (lambda o_, m_, i_: (lambda e: e.max_index(o_, m_, i_)))(tvu[:, gq, 8:16], tv[:, gq, 8:16], scr[:, 0:128]),
                 [b_scr, b_tv], [b_ti])
        k.cp("dve", tif[:, :, :], tvu[:, :, :], r=[b_ti], w=[b_tif])
        for h in range(8):
            c3 = cand.rearrange("p (a b) -> p a b", a=16)
            x3 = cidx.rearrange("p (a b) -> p a b", a=16)
            A0 = tv[:, 2 * h, :].unsqueeze(2).to_broadcast([128, 16, 16])
            B0 = tv[:, 2 * h + 1, :].unsqueeze(1).to_broadcast([128, 16, 16])
            k.tt("dve", c3, A0, B0, OP.add, r=[b_tv], w=[b_cand])
            k.ts("dve", x3, tif[:, 2 * h, :].unsqueeze(2).to_broadcast([128, 16, 16]), 128.0, None, OP.mult,
                 r=[b_tif], w=[b_cidx])
            k.tt("dve", x3, x3, tif[:, 2 * h + 1, :].unsqueeze(1).to_broadcast([128, 16, 16]), OP.add,
                 r=[b_cidx, b_tif], w=[b_cidx])
            S.op("dve", (lambda o_, i_: (lambda e: e.max(o_, i_)))(bv[:, h, 0:8], cand), [b_cand], [b_bv])
            S.op("dve", (lambda o_, m_, i_: (lambda e: e.match_replace(o_, m_, i_, NEG)))(scr, bv[:, h, 0:8], cand),
                 [b_cand, b_bv], [b_scr])
            S.op("dve", (lambda o_, i_: (lambda e: e.max(o_, i_)))(bv[:, h, 8:16], scr), [b_scr], [b_bv])
            for kq in range(16):
                slot = h * 16 + kq
                k.stt(scr, cand, bv[:, h, kq:kq + 1], cidx, OP.is_equal, OP.mult,
                      r=[b_cand, b_bv, b_cidx], w=[b_scr, b_ef], accum_out=ef[:, slot:slot + 1])
        k.ts("dve", fz, fz, 1.0, None, OP.mult, r=[b_fz], w=[b_fz, b_ef])
        k.ts("dve", ef, ef, 0.0, 16383.0, OP.max, OP.min, r=[b_ef], w=[b_ef])
        k.cp("dve", eidx[:, :], ef, r=[b_ef], w=[b_eidx])
        negm = stat[:, 8:16]
        k.ts("dve", negm, bv[:, :, 0], -1.0, None, OP.mult, r=[b_bv], w=[b_stat])
        for h in range(8):
            k.act(ex[:, h, :], bv[:, h, :], AF.Exp, r=[b_bv, b_stat], w=[b_ex, b_stat], bias=negm[:, h:h + 1],
                  accum_out=stat[:, 16 + h:17 + h])
        k.recip(stat[:, 24:32], stat[:, 16:24], r=[b_stat], w=[b_stat])
        k.tt("dve", ex[:, :, :], ex[:, :, :], stat[:, 24:32].unsqueeze(2).to_broadcast([128, 8, 16]), OP.mult,
             r=[b_ex, b_stat], w=[b_ex])
        for slot in range(128):
            bi = slot % NB
            S.dma("pool", (lambda o_, c_: (lambda e: e.indirect_dma_start(
                o_, None, eu_d[:, :], bass.IndirectOffsetOnAxis(ap=eidx[:, c_:c_ + 1], axis=0))))(ub[bi], slot),
                [b_eidx], [b_ub[bi]])
            k.stt(junk, ub[bi], 1.0, h2, OP.mult, OP.mult, r=[b_ub[bi], b_h2], w=[b_junk, b_dots],
                  accum_out=dots[:, slot:slot + 1])
        k.ts("dve", fz, fz, 1.0, None, OP.mult, r=[b_fz], w=[b_fz, b_dots])
        k.tt("dve", g1, dots, dots, OP.mult, r=[b_dots], w=[b_g])
        k.ts("dve", g1, g1, 0.044715, 1.0, OP.mult, OP.add, r=[b_g], w=[b_g])
        k.tt("dve", g1, g1, dots, OP.mult, r=[b_g, b_dots], w=[b_g])
        k.act(g2, g1, AF.Sigmoid, r=[b_g], w=[b_g], scale=1.5957691216057308)
        k.tt("dve", g2, g2, dots, OP.mult, r=[b_g, b_dots], w=[b_g])
        k.tt("dve", wgt, g2, ex[:, :, :].rearrange("p h n -> p (h n)"), OP.mult, r=[b_g, b_ex], w=[b_wgt])
        for slot in range(128):
            bi = slot % NB
            S.dma("pool", (lambda o_, c_: (lambda e: e.indirect_dma_start(
                o_, None, ev_d[:, :], bass.IndirectOffsetOnAxis(ap=eidx[:, c_:c_ + 1], axis=0))))(vb[bi], slot),
                [b_eidx], [b_vb[bi]])
            vs = slot % 2
            k.act(vbf[vs], vb[bi], AF.Copy, r=[b_vb[bi]], w=[b_vbf[vs]])
            k.act(dg[vs], ident, AF.Copy, r=[b_const, b_wgt], w=[b_dg[vs]], scale=wgt[:, slot:slot + 1])
            for half in range(2):
                k.mm(PS[4 + half][:, :], dg[vs], vbf[vs][:, half * 512:(half + 1) * 512], slot == 0, slot == 127,
                     r=[b_dg[vs], b_vbf[vs]], w=[PB[4 + half]])
        for half in range(2):
            cs = slice(half * 512, (half + 1) * 512)
            k.tt("dve", acc[:, cs], PS[4 + half][:, :], x1t[:, cs], OP.add, r=[PB[4 + half], b_x1t], w=[b_acc])
        ss2, sq2, rs2 = stat[:, 3:4], stat[:, 4:5], stat[:, 5:6]
        k.act(junk, acc, AF.Square, r=[b_acc], w=[b_junk, b_stat], accum_out=ss2)
        k.act(sq2, ss2, AF.Sqrt, r=[b_stat], w=[b_stat], bias=EPS, scale=1.0 / D)
        k.recip(rs2, sq2, r=[b_stat], w=[b_stat])
        k.stt(acc, acc, rs2, fnw_bc, OP.mult, OP.mult, r=[b_acc, b_stat, b_c3], w=[b_acc])
        k.ld("sp", yout[i * 128:(i + 1) * 128, :], acc, r=[b_acc], w=[b_y])


def build_nc(nt1=NT1_FULL, nt2=NT2_FULL, stage=2, cut=0, mock=False):
    nc = bass.Bass("TRN2", target_bir_lowering=False)
    dt_in = {}

    def din(name, shape, dt=F32):
        t = nc.dram_tensor(name, list(shape), dt, kind="ExternalInput")
        dt_in[name] = t
        return t.ap()

    xb = din("xb", [nt1 * 128, D])
    w1 = din("w1", [D, 2816])
    nmw = din("nmw", [128, 8])
    lbr = din("lbr", [2, 256])
    lbc = din("lbc", [128, 4])
    hnw = din("hnw", [1, 256])
    ident_d = din("ident", [128, 128], BF)
    rot_d = din("rot", [128, nt1, 384])
    dmaskT_d = din("dmaskT", [128, 128])
    qdec_d = din("qdec", [128, 128])
    cols_d = din("cols", [128, 4])
    trimid_d = din("trimid", [128, 130], BF)
    causT_d = din("causT", [128, 128])

    obuf = nc.dram_tensor("obuf", [SEQ, 768], BF)
    if stage == 1:
        y1 = nc.dram_tensor("y1", [nt1 * 128, 768], BF, kind="ExternalOutput").ap()
    else:
        gath = nc.dram_tensor("gath", [8 * SEQ, 768], BF)
        x1buf = nc.dram_tensor("x1buf", [nt2 * 128, D], F32)
        xo = din("xo", [nt2 * 128, D])
        oidx_d = din("oidx", [128, 64], U32)
        wg_d = din("wg", [D, 2048])
        wbh_d = din("wbh", [D, D])
        wbr_d = din("wbr", [2048, D])
        wo_d = din("wo", [D, D])
        nfw_d = din("nfw", [1, D])
        fnw_d = din("fnw", [1, D])
        wq_d = din("wq", [D, 2048])
        skT_d = din("skT", [128, 2048])
        eu_d = din("eu", [16384, D])
        ev_d = din("ev", [16384, D])
        iota_d = din("iota", [128, 128])
        yout = nc.dram_tensor("y", [nt2 * 128, D], F32, kind="ExternalOutput").ap()

    es = ExitStack()
    with es:
        S = Sched(nc, es)
        k = K(S)
        a16 = Arena(es.enter_context(nc.sbuf_tensor("a16", [128, 61000], BF)), 61000)
        a32 = Arena(es.enter_context(nc.sbuf_tensor("a32", [128, 17200], F32)), 17200)
        PS = [es.enter_context(nc.psum_tensor("ps%d" % i, [128, 512], F32)) for i in range(8)]
        PB = [Buf(excl=True) for _ in range(8)]

        ident = a16.get(128)
        b_const = Buf()
        k.ld("sp", ident, ident_d[:, :], w=[b_const])
        nmw_t = a32.get(8)
        k.ld("sp", nmw_t, nmw[:, :], w=[b_const])

        a16.mark, a32.mark = a16.off, a32.off
        wreg = a16.get(8 * 2816).rearrange("p (k n) -> p k n", k=8)
        b_w = Buf()
        stg = [a32.get(1408) for _ in range(2)]
        b_stg = [Buf(), Buf()]
        w1v = w1.rearrange("(k p) n -> p k n", p=128)
        ci = 0
        for kk in range(8):
            for hf in range(2):
                s = ci % 2
                k.ld("sp", stg[s], w1v[:, kk, hf * 1408:(hf + 1) * 1408], w=[b_stg[s]])
                eng = ("dve", "pool", "act")[ci % 3]
                dst = wreg[:, kk, hf * 1408:(hf + 1) * 1408]
                if eng == "act":
                    k.act(dst, stg[s], AF.Copy, r=[b_stg[s]], w=[b_w])
                else:
                    k.cp(eng, dst, stg[s], r=[b_stg[s]], w=[b_w])
                ci += 1

        dmaskT = a32.get(128)
        qdec = a32.get(128)
        cols = a32.get(4)
        trimid = a16.get(130)
        g_hi = a16.get(256)
        g_lo = a16.get(256)
        b_ghl = Buf()
        causT = a32.get(128)
        lb_bc = a32.get(256)
        oml_bc = a32.get(256)
        lbtmp = a32.get(512).rearrange("p (a n) -> p a n", a=2)
        nw_bc = a32.get(256)
        lbc_t = a32.get(4)
        lbcol = a32.get(2)
        omlcol = a32.get(2)
        k.ld("sp", dmaskT, dmaskT_d[:, :], w=[b_const])
        k.ld("sp", qdec, qdec_d[:, :], w=[b_const])
        k.ld("sp", cols, cols_d[:, :], w=[b_const])
        k.ld("sp", trimid, trimid_d[:, :], w=[b_const])
        k.ld("sp", causT, causT_d[:, :], w=[b_const])
        k.ld("sp", lbc_t, lbc[:, :], w=[b_const])
        b_lb = Buf()
        for a in range(2):
            k.ld("sp", lbtmp[:, a, :], lbr[a:a + 1, :].partition_broadcast(128), w=[b_lb])
        k.ld("sp", nw_bc, hnw[0:1, :].partition_broadcast(128), w=[b_lb])
        k.tt("dve", lb_bc, lbtmp[:, 0, :], lbtmp[:, 1, :], OP.subtract, r=[b_lb], w=[b_lb])
        k.act(lb_bc, lb_bc, AF.Sigmoid, r=[b_lb], w=[b_lb])
        k.ts("dve", oml_bc, lb_bc, -1.0, 1.0, OP.mult, OP.add, r=[b_lb], w=[b_lb])
        k.tt("dve", lbcol, lbc_t[:, 0:2], lbc_t[:, 2:4], OP.subtract, r=[b_const], w=[b_lb])
        k.act(lbcol, lbcol, AF.Sigmoid, r=[b_lb], w=[b_lb])
        k.ts("dve", omlcol, lbcol, -1.0, 1.0, OP.mult, OP.add, r=[b_lb], w=[b_lb])

        xt = [a32.get(1024) for _ in range(2)]
        b_xt = [Buf(), Buf()]
        rot = [a32.get(384) for _ in range(2)]
        b_rot = [Buf(), Buf()]
        sqj = a16.get(1024)
        xn = a16.get(1024)
        hT = a16.get(1024).rearrange("p (k t) -> p k t", k=8)
        b_xn, b_hT, b_sq = Buf(), Buf(), Buf()
        stat = a32.get(16)
        b_stat = Buf()
        M1 = a32.get(512)
        M2 = a32.get(512)
        b_M = Buf()
        qk = a16.get(512).rearrange("p (h w t) -> p h w t", h=2, w=2)
        b_qk = Buf()
        At = a16.get(128)
        b_At = Buf()
        kdec = a16.get(256)
        b_kdec = Buf()
        v_r = a16.get(512)
        b_vr = Buf()
        qdT = a16.get(256).rearrange("p (h t) -> p h t", h=2)
        b_qdT = Buf()
        Sr = [a32.get(512) for _ in range(2)]
        Sbf = [a16.get(512) for _ in range(2)]
        b_Sr = [Buf(), Buf()]
        b_Sbf = [Buf(), Buf()]
        sig = a32.get(512)
        sg = a32.get(512)
        b_sig, b_sg = Buf(), Buf()
        sig_tm = a32.get(256)
        g_tm = a32.get(256)
        sgn = a32.get(256)
        b_tm, b_g, b_sgn = Buf(), Buf(), Buf()
        EB = a32.get(256).rearrange("p (j t) -> p j t", j=2)
        ENB = a32.get(260).rearrange("p (j t) -> p j t", j=2)
        b_EB, b_ENB = Buf(), Buf()
        qdTh = a16.get(256).rearrange("p (j t) -> p j t", j=2)
        kdTh = a16.get(256).rearrange("p (j t) -> p j t", j=2)
        b_qdTh, b_kdTh = Buf(), Buf()
        Ath = a16.get(256).rearrange("p (j t) -> p j t", j=2)
        b_Ath = Buf()
        vh = a16.get(256)
        b_vh = Buf()
        kdt = a16.get(256)
        b_kdt = Buf()
        Sh = a32.get(256).rearrange("p (j t) -> p j t", j=2)
        b_Sh = [Buf(), Buf()]
        Spb = a16.get(256).rearrange("p (j t) -> p j t", j=2)
        b_Spb = [Buf(), Buf()]
        tmpS = a32.get(256).rearrange("p (j t) -> p j t", j=2)
        b_tmpS = [Buf(), Buf()]
        sigh = a32.get(256)
        ghw = a32.get(256)
        b_sigh, b_ghw = Buf(), Buf()
        ost = [a16.get(768) for _ in range(2)]
        b_ost = [Buf(), Buf()]
        b_obuf = Buf()

        for h in range(2):
            S.op("pool", (lambda ap: (lambda e: e.memset(ap, 0.0)))(Sr[h]), (), [b_Sr[h]])
            S.op("pool", (lambda ap: (lambda e: e.memset(ap, 0.0)))(Sbf[h]), (), [b_Sbf[h]])
            S.op("pool", (lambda ap: (lambda e: e.memset(ap, 0.0)))(Sh[:, h, :]), (), [b_Sh[h]])

        def load_tile(i):
            s = i % 2
            k.ld("sp", xt[s], xb[i * 128:(i + 1) * 128, :], w=[b_xt[s]])
            k.ld("sp", rot[s], rot_d[:, i, :], w=[b_rot[s]])

        load_tile(0)
        for i in range(nt1):
            s = i % 2
            if i + 1 < nt1:
                load_tile(i + 1)
            ss, sq_, rstd = stat[:, 0:1], stat[:, 1:2], stat[:, 2:3]
            k.act(sqj, xt[s], AF.Square, r=[b_xt[s]], w=[b_sq, b_stat], accum_out=ss)
            k.act(sq_, ss, AF.Sqrt, r=[b_stat], w=[b_stat], bias=EPS, scale=1.0 / D)
            k.recip(rstd, sq_, r=[b_stat], w=[b_stat])
            k.ts("dve", xn, xt[s], rstd, None, OP.mult, r=[b_xt[s], b_stat], w=[b_xn])
            if cut == 1:
                break
            for c in range(8):
                pb = c // 4
                k.mm(PS[pb][:, (c % 4) * 128:(c % 4 + 1) * 128], xn[:, c * 128:(c + 1) * 128], ident,
                     True, True, r=[b_xn, b_const], w=[PB[pb]])
            for c in range(8):
                pb = c // 4
                src = PS[pb][:, (c % 4) * 128:(c % 4 + 1) * 128]
                if c % 2 == 0:
                    k.act(hT[:, c, :], src, AF.Copy, r=[PB[pb], b_const], w=[b_hT], scale=nmw_t[:, c:c + 1])
                else:
                    k.ts("dve", hT[:, c, :], src, nmw_t[:, c:c + 1], None, OP.mult,
                         r=[PB[pb], b_const], w=[b_hT])
            if cut == 2:
                break
            for j in range(8):
                pb = 2 + j // 4
                for kk in range(8):
                    k.mm(PS[pb][:, (j % 4) * 128:(j % 4 + 1) * 128], wreg[:, kk, j * 128:(j + 1) * 128],
                         hT[:, kk, :], kk == 0, kk == 7, r=[b_w, b_hT], w=[PB[pb]])
            for j, (pb, c0, n) in enumerate(((4, 1024, 512), (5, 1536, 512), (6, 2048, 512), (7, 2560, 256))):
                for kk in range(8):
                    k.mm(PS[pb][:, 0:n], hT[:, kk, :], wreg[:, kk, c0:c0 + n], kk == 0, kk == 7,
                         r=[b_w, b_hT], w=[PB[pb]])

            if cut == 3:
                break
            rs = rot[s].rearrange("p (a t) -> p a t", a=3)
            F2 = PS[3][:, :].rearrange("p (w h t) -> p w h t", w=2, h=2)
            M1v = M1.rearrange("p (w h t) -> p w h t", w=2, h=2)
            M2v = M2.rearrange("p (w h t) -> p w h t", w=2, h=2)
            for wv in range(2):
                k.tt("dve", M1v[:, wv, :, :], F2[:, wv, :, :], rs[:, 0:2, :], OP.mult,
                     r=[PB[3], b_rot[s]], w=[b_M])
                k.tt("dve", M2v[:, wv, :, :], F2[:, wv, :, :], rs[:, 1:3, :], OP.mult,
                     r=[PB[3], b_rot[s]], w=[b_M])
            k.tt("pool", qk[:, 0, :, :], M1v[:, :, 0, :], M1v[:, :, 1, :], OP.subtract, r=[b_M], w=[b_qk])
            k.tt("pool", qk[:, 1, :, :], M2v[:, :, 0, :], M2v[:, :, 1, :], OP.add, r=[b_M], w=[b_qk])
            if cut == 4:
                break
            k.act(v_r, PS[5][:, :], AF.Copy, r=[PB[5]], w=[b_vr])
            k.act(sig, PS[6][:, :], AF.Sigmoid, r=[PB[6]], w=[b_sig])
            k.tt("dve", sg, PS[6][:, :], sig, OP.mult, r=[PB[6], b_sig], w=[b_sg])
            if cut == 5:
                break
            for h in range(2):
                k.mm(PS[0][:, 0:128], qk[:, h, 1, :], qk[:, h, 0, :], h == 0, h == 1,
                     r=[b_qk], w=[PB[0]])
            for h in range(2):
                k.mm(PS[1][:, h * 128:(h + 1) * 128], qk[:, h, 1, :], ident, True, True,
                     r=[b_qk, b_const], w=[PB[1]])
            k.tt("dve", At, PS[0][:, 0:128], dmaskT, OP.mult, r=[PB[0], b_const], w=[b_At])
            k.act(kdec, PS[1][:, 0:256], AF.Copy, r=[PB[1], b_const], w=[b_kdec], scale=cols[:, 0:1])
            k.tt("pool", qdT[:, 0, :], qk[:, 0, 0, :], qdec, OP.mult, r=[b_qk, b_const], w=[b_qdT])
            k.tt("pool", qdT[:, 1, :], qk[:, 1, 0, :], qdec, OP.mult, r=[b_qk, b_const], w=[b_qdT])
            k.mm(PS[0][:, :], At, v_r, True, False, r=[b_At, b_vr], w=[PB[0]])
            for h in range(2):
                k.mm(PS[0][:, :], qdT[:, h, :], Sbf[h], False, h == 1, r=[b_qdT, b_Sbf[h]], w=[PB[0]])
            if cut == 6:
                break
            for h in range(2):
                pb = 1 if h == 0 else 5
                k.mm(PS[pb][:, :], kdec[:, h * 128:(h + 1) * 128], v_r, True, True,
                     r=[b_kdec, b_vr], w=[PB[pb]])
                k.stt(Sr[h], Sr[h], cols[:, 1:2], PS[pb][:, :], OP.mult, OP.add,
                      r=[b_Sr[h], PB[pb], b_const], w=[b_Sr[h]])
                k.cp("pool", Sbf[h], Sr[h], r=[b_Sr[h]], w=[b_Sbf[h]])
            ssr, sqr, rr = stat[:, 3:4], stat[:, 4:5], stat[:, 5:6]
            k.act(sqj[:, 0:512], PS[0][:, :], AF.Square, r=[PB[0]], w=[b_sq, b_stat], accum_out=ssr)
            k.act(sqr, ssr, AF.Sqrt, r=[b_stat], w=[b_stat], bias=EPS, scale=1.0 / 512)
            k.recip(rr, sqr, r=[b_stat], w=[b_stat])
            k.stt(ost[s][:, 256:768], PS[0][:, :], rr, sg, OP.mult, OP.mult,
                  r=[PB[0], b_stat, b_sg], w=[b_ost[s]])

            if cut == 7:
                break
            k.act(sig_tm, PS[4][:, 0:256], AF.Sigmoid, r=[PB[4]], w=[b_tm])
            k.tt("pool", sig_tm, sig_tm, oml_bc, OP.mult, r=[b_tm, b_lb], w=[b_tm])
            k.tt("pool", sig_tm, sig_tm, lb_bc, OP.add, r=[b_tm, b_lb], w=[b_tm])
            k.act(g_tm, sig_tm, AF.Ln, r=[b_tm], w=[b_g])
            k.cp("dve", vh, PS[4][:, 256:512], r=[PB[4]], w=[b_vh])
            k.act(sgn, PS[2][:, 256:512], AF.Sigmoid, r=[PB[2]], w=[b_sgn], scale=-1.0)
            k.act(sigh, PS[7][:, 0:256], AF.Sigmoid, r=[PB[7]], w=[b_sigh])
            k.tt("dve", ghw, PS[7][:, 0:256], sigh, OP.mult, r=[PB[7], b_sigh], w=[b_ghw])
            k.tt("pool", ghw, ghw, nw_bc, OP.mult, r=[b_ghw, b_lb], w=[b_ghw])
            if cut == 8:
                break
            k.cp("dve", g_hi, g_tm, r=[b_g], w=[b_ghl])
            k.tt("dve", g_lo, g_tm, g_hi, OP.subtract, r=[b_g, b_ghl], w=[b_ghl])
            for j in range(2):
                k.mm(PS[6][:, j * 256:j * 256 + 130], g_hi[:, j * 128:(j + 1) * 128], trimid, True, False,
                     r=[b_ghl, b_const], w=[PB[6]])
                k.mm(PS[6][:, j * 256:j * 256 + 130], g_lo[:, j * 128:(j + 1) * 128], trimid, False, True,
                     r=[b_ghl, b_const], w=[PB[6]])
            for j in range(2):
                k.act(EB[:, j, :], PS[6][:, j * 256:j * 256 + 128], AF.Exp, r=[PB[6]], w=[b_EB])
                k.act(ENB[:, j, :], PS[6][:, j * 256:j * 256 + 130], AF.Exp, r=[PB[6]], w=[b_ENB], scale=-1.0)
            for j in range(2):
                k.stt(qdTh[:, j, :], PS[2][:, j * 128:(j + 1) * 128], float(128 ** -0.5), EB[:, j, :],
                      OP.mult, OP.mult, r=[PB[2], b_EB], w=[b_qdTh])
                k.stt(kdTh[:, j, :], sgn[:, j * 128:(j + 1) * 128], omlcol[:, j:j + 1], ENB[:, j, 0:128],
                      OP.mult, OP.mult, r=[b_sgn, b_ENB, b_lb], w=[b_kdTh])
            if cut == 9:
                break
            for j in range(2):
                k.mm(PS[7][:, 256 + j * 128:256 + (j + 1) * 128], kdTh[:, j, :], qdTh[:, j, :], True, True,
                     r=[b_kdTh, b_qdTh], w=[PB[7]])
            for j in range(2):
                k.tt("dve", Ath[:, j, :], PS[7][:, 256 + j * 128:256 + (j + 1) * 128], causT, OP.mult,
                     r=[PB[7], b_const], w=[b_Ath])
            for j in range(2):
                k.mm(PS[3][:, j * 128:(j + 1) * 128], kdTh[:, j, :], ident, True, True,
                     r=[b_kdTh, b_const], w=[PB[3]])
            k.cp("dve", kdt, PS[3][:, 0:256], r=[PB[3]], w=[b_kdt])
            for j in range(2):
                k.ts("dve", Spb[:, j, :], Sh[:, j, :], ENB[:, j, 128:129], None, OP.mult,
                     r=[b_Sh[j], b_ENB], w=[b_Spb[j]])
            if cut == 10:
                break
            for j in range(2):
                k.mm(PS[4][:, j * 128:(j + 1) * 128], Ath[:, j, :], vh[:, j * 128:(j + 1) * 128], True, False,
                     r=[b_Ath, b_vh], w=[PB[4]])
                k.mm(PS[4][:, j * 128:(j + 1) * 128], qdTh[:, j, :], Spb[:, j, :], False, True,
                     r=[b_qdTh, b_Spb[j]], w=[PB[4]])
            for j in range(2):
                k.mm(PS[3][:, 256 + j * 128:256 + (j + 1) * 128], kdt[:, j * 128:(j + 1) * 128],
                     vh[:, j * 128:(j + 1) * 128], True, True, r=[b_kdt, b_vh], w=[PB[3]])
            for j in range(2):
                k.stt(tmpS[:, j, :], Sh[:, j, :], ENB[:, j, 128:129], PS[3][:, 256 + j * 128:256 + (j + 1) * 128],
                      OP.mult, OP.add, r=[b_Sh[j], b_ENB, PB[3]], w=[b_tmpS[j]])
                k.ts("dve", Sh[:, j, :], tmpS[:, j, :], EB[:, j, 127:128], None, OP.mult,
                     r=[b_tmpS[j], b_EB], w=[b_Sh[j]])
            for j in range(2):
                ssh, sqh, rh = stat[:, 6 + 3 * j:7 + 3 * j], stat[:, 7 + 3 * j:8 + 3 * j], stat[:, 8 + 3 * j:9 + 3 * j]
                k.act(sqj[:, 512 + j * 128:512 + (j + 1) * 128], PS[4][:, j * 128:(j + 1) * 128], AF.Square,
                      r=[PB[4]], w=[b_sq, b_stat], accum_out=ssh)
                k.act(sqh, ssh, AF.Sqrt, r=[b_stat], w=[b_stat], bias=EPS, scale=1.0 / 128)
                k.recip(rh, sqh, r=[b_stat], w=[b_stat])
                k.stt(ost[s][:, j * 128:(j + 1) * 128], PS[4][:, j * 128:(j + 1) * 128], rh,
                      ghw[:, j * 128:(j + 1) * 128], OP.mult, OP.mult,
                      r=[PB[4], b_stat, b_ghw], w=[b_ost[s]])
            if stage == 1:
                k.ld("sp", y1[i * 128:(i + 1) * 128, :], ost[s], r=[b_ost[s]], w=[b_obuf])
            else:
                k.ld("sp", obuf[i * 128:(i + 1) * 128, :], ost[s], r=[b_ost[s]], w=[b_obuf])


        if stage == 2 and not cut:
            phase2(nc, es, S, k, a16, a32, PS, PB, ident, b_const, obuf, b_obuf, gath, x1buf, xo, oidx_d,
                   wg_d, wbh_d, wbr_d, wo_d, nmw_t, nfw_d, fnw_d, wq_d, skT_d, eu_d, ev_d, iota_d, yout, nt2, mock)
        if cut:
            k.ld("sp", y1[0:128, :], ost[0], r=[b_ost[0]], w=[b_obuf])
        S.finish()
        with nc.Block() as block:
            S.emit(block)
    return nc


def _bf(a):
    return np.ascontiguousarray(a).astype(ml_dtypes.bfloat16)


def make_inputs2(core, x, w_in, w_branch_hg, w_branch_ret, w_out, norm_ffn_w, peer_w_q, peer_sub_keys,
                 expert_u, expert_v, final_norm_w, **kw):
    b, g = divmod(core, 4)
    p = np.arange(128, dtype=np.int64)[:, None]
    col = np.arange(64, dtype=np.int64)[None, :]
    i_, r_ = col // 4, col % 4
    oidx = ((b * 4 + r_) * SEQ + g * 2048 + i_ * 128 + p).astype(np.uint32)
    f = lambda a: np.ascontiguousarray(a, dtype=np.float32)
    skT = f(np.transpose(peer_sub_keys[0].reshape(16, 128, 128), (2, 0, 1)).reshape(128, 2048))
    return {
        "xo": f(x[b, g * 2048:(g + 1) * 2048]),
        "oidx": np.ascontiguousarray(oidx),
        "wg": f(w_in[0][:, 10240:12288]),
        "wbh": f(w_branch_hg[0]), "wbr": f(w_branch_ret[0]), "wo": f(w_out[0]),
        "nfw": f(norm_ffn_w[0].reshape(1, D)), "fnw": f(final_norm_w.reshape(1, D)),
        "wq": f(peer_w_q[0]), "skT": skT,
        "eu": f(expert_u[0]), "ev": f(expert_v[0]),
        "iota": f(np.broadcast_to(np.arange(128, dtype=np.float32)[None, :], (128, 128))),
    }


def make_inputs(core, x, norm_mix_w, w_in, hg_lower_bounds, hg_norm_w, **kw):
    b, g = divmod(core, 4)
    W = w_in[0]
    h0, h1 = 2 * g, 2 * g + 1

    def hcol(base, h, n=128):
        return W[:, base + h * n: base + (h + 1) * n]
    HQ, HF, HI, HG = 0, 1024, 2048, 3072
    RQ, RK, RV, RG = 4096, 5120, 6144, 8192
    fm = [hcol(HQ, h0), hcol(HQ, h1), hcol(HF, h0), hcol(HF, h1),
          W[:, RQ + g * 256: RQ + (g + 1) * 256], W[:, RK + g * 256: RK + (g + 1) * 256]]
    tm = [hcol(HF, h0), hcol(HF, h1), hcol(HI, h0), hcol(HI, h1),
          W[:, RV + g * 512: RV + (g + 1) * 512], W[:, RG + g * 512: RG + (g + 1) * 512],
          hcol(HG, h0), hcol(HG, h1)]
    w1 = np.ascontiguousarray(np.concatenate(fm + tm, axis=1), dtype=np.float32)
    assert w1.shape == (1024, 2816)
    lb = hg_lower_bounds
    lbr = np.ascontiguousarray(lb[:, h0 * 128:(h1 + 1) * 128], dtype=np.float32)
    lbc = np.ascontiguousarray(np.stack([lb[0, h0 * 128:(h0 + 1) * 128], lb[0, h1 * 128:(h1 + 1) * 128],
                                         lb[1, h0 * 128:(h0 + 1) * 128], lb[1, h1 * 128:(h1 + 1) * 128]], axis=1),
                               dtype=np.float32)
    inv = (10000.0 ** (-np.arange(128, dtype=np.float32) / np.float32(128))).astype(np.float32)
    pos = np.arange(SEQ, dtype=np.float32)
    ang = (pos[None, :] * inv[:, None]).astype(np.float32)
    cosT = np.cos(ang).astype(np.float32).reshape(128, NT1_FULL, 128)
    sinT = np.sin(ang).astype(np.float32).reshape(128, NT1_FULL, 128)
    rot = np.ascontiguousarray(np.stack([cosT, sinT, cosT], axis=2).reshape(128, NT1_FULL, 384))
    log_g = np.log(np.float32(1.0) - np.exp2(np.float32(-5.0 - g))).astype(np.float32)
    idx = np.arange(128, dtype=np.float32)
    rel = idx[None, :] - idx[:, None]
    dmaskT = np.where(rel >= 0, np.exp(log_g * np.maximum(rel, 0.0)), 0.0).astype(np.float32) / np.float32(16.0)
    qdec = np.broadcast_to(np.exp(log_g * (idx + 1.0)).astype(np.float32)[None, :], (128, 128)).copy()
    cols = np.zeros((128, 4), np.float32)
    cols[:, 0] = np.exp(log_g * (127.0 - idx)) / 16.0
    cols[:, 1] = np.exp(log_g * 128.0)
    tri = np.zeros((128, 130), np.float32)
    sidx = np.arange(128)
    for t in range(128):
        tri[:, t] = (sidx <= t).astype(np.float32) - (sidx <= 63).astype(np.float32)
    tri[:, 128] = -(sidx <= 63).astype(np.float32)
    causT = (sidx[:, None] <= sidx[None, :]).astype(np.float32)
    return {
        "xb": np.ascontiguousarray(x[b], dtype=np.float32),
        "w1": w1,
        "nmw": np.ascontiguousarray(norm_mix_w[0].reshape(8, 128).T, dtype=np.float32),
        "lbr": lbr, "lbc": lbc,
        "hnw": np.ascontiguousarray(hg_norm_w[0, h0 * 128:(h1 + 1) * 128].reshape(1, 256), dtype=np.float32),
        "ident": _bf(np.eye(128, dtype=np.float32)),
        "rot": rot, "dmaskT": dmaskT, "qdec": qdec, "cols": cols, "trimid": _bf(tri), "causT": causT,
    }


def kernel(**inputs):
    inputs = {k_: np.asarray(v) for k_, v in inputs.items()}
    nc = build_nc()
    in_maps = []
    for c in range(NCORE):
        m = make_inputs(c, **inputs)
        m.update(make_inputs2(c, **inputs))
        in_maps.append(m)
    res = run_bass_kernel_spmd(nc, in_maps, core_ids=list(range(NCORE)))
    out = np.zeros((2, SEQ, D), np.float32)
    for c in range(NCORE):
        b, g = divmod(c, 4)
        out[b, g * 2048:(g + 1) * 2048] = res.results[c]["y"]
    return out
```
